# Optimizing a Trainium2 kernel written in Bass

```python
import math
import jax
import jax.numpy as jnp
from jax import lax
import numpy as np

D_MODEL = 1024
BATCH = 8
SEQ = 2048
DEPTH = 2

GLA_HEADS = 4
GLA_DK = 64
GLA_DV = 128
GLA_GATE_RANK = 16
GLA_GATE_TEMP = 16.0
GLA_CHUNK = 64
DIFF_HEADS = 4
DIFF_DK = 64
DIFF_DV = 128
N_BUCKETS = 32
MAX_DISTANCE = 128
MLA_HEADS = 16
MLA_Q_LORA = 256
MLA_KV_LORA = 128
MLA_NOPE = 64
MLA_ROPE = 32
MLA_DV = 64
ROPE_THETA = 10000.0
D_FF = 2816
N_EXPERTS = 8
TOP_K = 2
Q_BLOCK = 128
DEEPNORM_ALPHA = (2 * DEPTH) ** 0.25
DEEPNORM_BETA = (8 * DEPTH) ** -0.25
N_EVEN = (DEPTH + 1) // 2
N_ODD = DEPTH // 2
EVEN_IN_WIDTHS = (GLA_HEADS * GLA_DK, GLA_HEADS * GLA_DK, GLA_HEADS * GLA_DV, GLA_HEADS * GLA_DV,
                  2 * GLA_GATE_RANK,
                  DIFF_HEADS * 2 * DIFF_DK, DIFF_HEADS * 2 * DIFF_DK, DIFF_HEADS * DIFF_DV)
EVEN_IN = sum(EVEN_IN_WIDTHS)
EVEN_OUT = GLA_HEADS * GLA_DV + DIFF_HEADS * DIFF_DV
ODD_IN = MLA_Q_LORA + MLA_KV_LORA + MLA_ROPE
ODD_OUT = MLA_HEADS * MLA_DV

kernel_name = 'hybrid_gla_diff_mla_moe_encoder'


def _split_widths(t, widths):
    idx, acc = [], 0
    for w in widths[:-1]:
        acc += w
        idx.append(acc)
    return jnp.split(t, idx, axis=-1)


def _layer_norm(x, g, b, eps=1e-5):
    xf = x.astype(jnp.float32)
    mu = jnp.mean(xf, axis=-1, keepdims=True)
    var = jnp.mean(jnp.square(xf - mu), axis=-1, keepdims=True)
    y = (xf - mu) * lax.rsqrt(var + eps) * g.astype(jnp.float32) + b.astype(jnp.float32)
    return y.astype(x.dtype)


def _rms_norm(x, g, eps=1e-6):
    xf = x.astype(jnp.float32)
    y = xf * lax.rsqrt(jnp.mean(jnp.square(xf), axis=-1, keepdims=True) + eps) * g.astype(jnp.float32)
    return y.astype(x.dtype)


def _rope(t, positions):
    half = t.shape[-1] // 2
    inv = ROPE_THETA ** (-jnp.arange(half, dtype=jnp.float32) / half)
    ang = positions.astype(jnp.float32)[:, None] * inv[None, :]
    cos, sin = jnp.cos(ang), jnp.sin(ang)
    tf = t.astype(jnp.float32)
    t1, t2 = tf[..., :half], tf[..., half:]
    return jnp.concatenate([t1 * cos - t2 * sin, t1 * sin + t2 * cos], axis=-1).astype(t.dtype)


def _relative_bucket(rel):
    half = N_BUCKETS // 2
    max_exact = half // 2
    bucket = jnp.where(rel > 0, half, 0).astype(jnp.int32)
    n = jnp.abs(rel)
    n_large = max_exact + (jnp.log(jnp.maximum(n, max_exact).astype(jnp.float32) / max_exact)
                           / math.log(MAX_DISTANCE / max_exact) * (half - max_exact)).astype(jnp.int32)
    n_large = jnp.minimum(n_large, half - 1)
    return bucket + jnp.where(n < max_exact, n, n_large)


def _block_softmax_attention(q, k, v, map_weights, bias_table):
    B, H, M, S, dq = q.shape
    dv = v.shape[-1]
    nb = S // Q_BLOCK
    scale = dq ** -0.5
    q_blocks = jnp.moveaxis(q.reshape(B, H, M, nb, Q_BLOCK, dq), 3, 0)
    starts = jnp.arange(nb, dtype=jnp.int32) * Q_BLOCK
    k_pos = jnp.arange(S, dtype=jnp.int32)
    mw = map_weights.astype(jnp.float32)

    def one_block(args):
        qb, start = args
        s = jnp.einsum('bhmqd,bhmkd->bhmqk', qb, k).astype(jnp.float32) * scale
        if bias_table is not None:
            q_pos = start + jnp.arange(Q_BLOCK, dtype=jnp.int32)
            bucket = _relative_bucket(k_pos[None, :] - q_pos[:, None])
            bias = jnp.moveaxis(bias_table[bucket], -1, 0).astype(jnp.float32)
            s = s + bias[None, :, None]
        p = jax.nn.softmax(s, axis=-1)
        a = jnp.einsum('m,bhmqk->bhqk', mw, p)
        return jnp.einsum('bhqk,bhkd->bhqd', a.astype(v.dtype), v)

    out = lax.map(one_block, (q_blocks, starts))
    return jnp.moveaxis(out, 0, 2).reshape(B, H, S, dv)


def _gla_chunked(q, k, v, log_a):
    out_dtype = v.dtype
    B, H, S, dk = q.shape
    dv = v.shape[-1]
    n = S // GLA_CHUNK
    C = GLA_CHUNK
    q = q.astype(jnp.float32).reshape(B, H, n, C, dk)
    k = k.astype(jnp.float32).reshape(B, H, n, C, dk)
    v = v.astype(jnp.float32).reshape(B, H, n, C, dv)
    b = jnp.cumsum(log_a.astype(jnp.float32).reshape(B, H, n, C, dk), axis=3)
    b_last = b[:, :, :, -1:, :]
    q_dec = q * jnp.exp(b)
    mask = jnp.tril(jnp.ones((C, C), jnp.float32))
    attn = jnp.einsum('bhnid,bhnjd->bhnij', q_dec, k * jnp.exp(-b)) * mask
    o_intra = jnp.einsum('bhnij,bhnjv->bhniv', attn, v)
    chunk_kv = jnp.einsum('bhnjd,bhnjv->bhndv', k * jnp.exp(b_last - b), v)
    decay = jnp.exp(b_last[:, :, :, 0, :])

    def step(state, inp):
        dec, kv = inp
        return dec[..., None] * state + kv, state

    init = jnp.zeros((B, H, dk, dv), jnp.float32)
    _, s_before = lax.scan(step, init, (jnp.moveaxis(decay, 2, 0), jnp.moveaxis(chunk_kv, 2, 0)))
    s_before = jnp.moveaxis(s_before, 0, 2)
    o_inter = jnp.einsum('bhnid,bhndv->bhniv', q_dec, s_before)
    return (o_intra + o_inter).reshape(B, H, S, dv).astype(out_dtype)


def _even_mixer(x, w_in, gate_up, gate_bias, gla_gain, diff_lambda, diff_gain, w_out, rel_bias_table, lam_init):
    B, S, _ = x.shape
    q_g, k_g, v_g, g_g, a_lr, q_d, k_d, v_d = _split_widths(x @ w_in, EVEN_IN_WIDTHS)

    def heads(t, h, d):
        return t.reshape(B, S, h, d).transpose(0, 2, 1, 3)

    qg = heads(q_g, GLA_HEADS, GLA_DK) * (GLA_DK ** -0.5)
    kg = heads(k_g, GLA_HEADS, GLA_DK)
    vg = heads(v_g, GLA_HEADS, GLA_DV)
    a_lr = a_lr.reshape(B, S, 2, GLA_GATE_RANK)
    gate_logits = jnp.einsum('bsdr,drk->dbsk', a_lr, gate_up) + gate_bias[:, None, None, :]
    log_a = jax.nn.log_sigmoid(gate_logits.astype(jnp.float32)) / GLA_GATE_TEMP
    la_f = heads(log_a[0], GLA_HEADS, GLA_DK)
    la_b = heads(log_a[1], GLA_HEADS, GLA_DK)
    o_f = _gla_chunked(qg, kg, vg, la_f)
    flip = lambda t: jnp.flip(t, axis=2)
    o_b = flip(_gla_chunked(flip(qg), flip(kg), flip(vg), flip(la_b)))
    o_gla = _rms_norm(o_f + o_b, gla_gain)
    o_gla = o_gla.transpose(0, 2, 1, 3).reshape(B, S, GLA_HEADS * GLA_DV) * jax.nn.silu(g_g)

    qd = q_d.reshape(B, S, DIFF_HEADS, 2, DIFF_DK).transpose(0, 2, 3, 1, 4)
    kd = k_d.reshape(B, S, DIFF_HEADS, 2, DIFF_DK).transpose(0, 2, 3, 1, 4)
    vd = heads(v_d, DIFF_HEADS, DIFF_DV)
    lf = diff_lambda.astype(jnp.float32)
    lam = jnp.exp(jnp.sum(lf[0] * lf[1])) - jnp.exp(jnp.sum(lf[2] * lf[3])) + lam_init
    map_w = jnp.stack([jnp.ones((), jnp.float32), -lam])
    o_d = _block_softmax_attention(qd, kd, vd, map_w, rel_bias_table)
    o_d = _rms_norm(o_d, diff_gain) * (1.0 - lam_init)
    o_diff = o_d.transpose(0, 2, 1, 3).reshape(B, S, DIFF_HEADS * DIFF_DV)

    return jnp.concatenate([o_gla, o_diff], axis=-1) @ w_out


def _mla_mixer(x, w_in, q_gain, kv_gain, w_uq, w_ukv, w_out):
    B, S, _ = x.shape
    c_q, c_kv, k_r = _split_widths(x @ w_in, (MLA_Q_LORA, MLA_KV_LORA, MLA_ROPE))
    q = (_rms_norm(c_q, q_gain) @ w_uq).reshape(B, S, MLA_HEADS, MLA_NOPE + MLA_ROPE).transpose(0, 2, 1, 3)
    kv = (_rms_norm(c_kv, kv_gain) @ w_ukv).reshape(B, S, MLA_HEADS, MLA_NOPE + MLA_DV).transpose(0, 2, 1, 3)
    pos = jnp.arange(S, dtype=jnp.int32)
    q = jnp.concatenate([q[..., :MLA_NOPE], _rope(q[..., MLA_NOPE:], pos)], axis=-1)
    k_rope = jnp.broadcast_to(_rope(k_r, pos)[:, None], (B, MLA_HEADS, S, MLA_ROPE))
    k = jnp.concatenate([kv[..., :MLA_NOPE], k_rope], axis=-1)
    v = kv[..., MLA_NOPE:]
    o = _block_softmax_attention(q[:, :, None], k[:, :, None], v, jnp.ones((1,), jnp.float32), None)
    return o.transpose(0, 2, 1, 3).reshape(B, S, ODD_OUT) @ w_out


def _swiglu(x, w_gate, w_up, w_down):
    return (jax.nn.silu(x @ w_gate) * (x @ w_up)) @ w_down


def _moe_swiglu(x, router_w, w_gate, w_up, w_down):
    B, S, D = x.shape
    t = x.reshape(B * S, D)
    logits = (t @ router_w).astype(jnp.float32)
    top_vals, top_idx = lax.top_k(logits, TOP_K)
    w = jax.nn.softmax(top_vals, axis=-1)
    combine = jnp.einsum('nk,nke->ne', w, jax.nn.one_hot(top_idx, N_EXPERTS, dtype=jnp.float32))
    out = jnp.zeros_like(t)
    for e in range(N_EXPERTS):
        out = out + combine[:, e:e + 1].astype(t.dtype) * _swiglu(t, w_gate[e], w_up[e], w_down[e])
    return out.reshape(B, S, D)


def setup_inputs(seed: int = 0) -> dict:
    key = jax.random.key(seed)
    ks = iter(jax.random.split(key, 32))

    def dense(shape, fan_in, scale=1.0):
        return jax.random.normal(next(ks), shape, jnp.float32) * (scale * fan_in ** -0.5)

    def gain(shape):
        return 1.0 + 0.02 * jax.random.normal(next(ks), shape, jnp.float32)

    def small(shape, s):
        return s * jax.random.normal(next(ks), shape, jnp.float32)

    beta = DEEPNORM_BETA
    return {
        'x': jax.random.normal(next(ks), (BATCH, SEQ, D_MODEL), jnp.float32),
        'rel_bias_table': small((N_BUCKETS, DIFF_HEADS), 0.5),
        'even_w_in': dense((N_EVEN, D_MODEL, EVEN_IN), D_MODEL),
        'gla_gate_up': dense((N_EVEN, 2, GLA_GATE_RANK, GLA_HEADS * GLA_DK), GLA_GATE_RANK),
        'gla_gate_bias': small((N_EVEN, 2, GLA_HEADS * GLA_DK), 0.1),
        'gla_norm_gain': gain((N_EVEN, GLA_DV)),
        'diff_lambda': small((N_EVEN, 4, DIFF_DK), 0.1),
        'diff_norm_gain': gain((N_EVEN, DIFF_DV)),
        'even_w_out': dense((N_EVEN, EVEN_OUT, D_MODEL), EVEN_OUT, beta),
        'ffn_w_gate': dense((N_EVEN, D_MODEL, D_FF), D_MODEL),
        'ffn_w_up': dense((N_EVEN, D_MODEL, D_FF), D_MODEL),
        'ffn_w_down': dense((N_EVEN, D_FF, D_MODEL), D_FF, beta),
        'odd_w_in': dense((N_ODD, D_MODEL, ODD_IN), D_MODEL),
        'mla_q_norm_gain': gain((N_ODD, MLA_Q_LORA)),
        'mla_kv_norm_gain': gain((N_ODD, MLA_KV_LORA)),
        'mla_w_uq': dense((N_ODD, MLA_Q_LORA, MLA_HEADS * (MLA_NOPE + MLA_ROPE)), MLA_Q_LORA),
        'mla_w_ukv': dense((N_ODD, MLA_KV_LORA, MLA_HEADS * (MLA_NOPE + MLA_DV)), MLA_KV_LORA),
        'odd_w_out': dense((N_ODD, ODD_OUT, D_MODEL), ODD_OUT, beta),
        'router_w': dense((N_ODD, D_MODEL, N_EXPERTS), D_MODEL),
        'moe_w_gate': dense((N_ODD, N_EXPERTS, D_MODEL, D_FF), D_MODEL),
        'moe_w_up': dense((N_ODD, N_EXPERTS, D_MODEL, D_FF), D_MODEL),
        'moe_w_down': dense((N_ODD, N_EXPERTS, D_FF, D_MODEL), D_FF, beta),
        'ln_gain': gain((DEPTH, 2, D_MODEL)),
        'ln_bias': small((DEPTH, 2, D_MODEL), 0.02),
    }


def reference(x, rel_bias_table, even_w_in, gla_gate_up, gla_gate_bias, gla_norm_gain, diff_lambda,
              diff_norm_gain, even_w_out, ffn_w_gate, ffn_w_up, ffn_w_down, odd_w_in, mla_q_norm_gain,
              mla_kv_norm_gain, mla_w_uq, mla_w_ukv, odd_w_out, router_w, moe_w_gate, moe_w_up, moe_w_down,
              ln_gain, ln_bias):
    for layer in range(DEPTH):
        i = layer // 2
        if layer % 2 == 0:
            lam_init = 0.8 - 0.6 * math.exp(-0.3 * layer)
            h = _even_mixer(x, even_w_in[i], gla_gate_up[i], gla_gate_bias[i], gla_norm_gain[i],
                            diff_lambda[i], diff_norm_gain[i], even_w_out[i], rel_bias_table, lam_init)
            x = _layer_norm(DEEPNORM_ALPHA * x + h, ln_gain[layer, 0], ln_bias[layer, 0])
            f = _swiglu(x, ffn_w_gate[i], ffn_w_up[i], ffn_w_down[i])
        else:
            h = _mla_mixer(x, odd_w_in[i], mla_q_norm_gain[i], mla_kv_norm_gain[i], mla_w_uq[i],
                           mla_w_ukv[i], odd_w_out[i])
            x = _layer_norm(DEEPNORM_ALPHA * x + h, ln_gain[layer, 0], ln_bias[layer, 0])
            f = _moe_swiglu(x, router_w[i], moe_w_gate[i], moe_w_up[i], moe_w_down[i])
        x = _layer_norm(DEEPNORM_ALPHA * x + f, ln_gain[layer, 1], ln_bias[layer, 1])
    return x
```

```python
import math
from contextlib import ExitStack

import numpy as np
import concourse.bass as bass
import concourse.mybir as mybir
from concourse.bass_utils import run_bass_kernel_spmd

F32 = mybir.dt.float32
BF16 = mybir.dt.bfloat16
AF = mybir.ActivationFunctionType
ALU = mybir.AluOpType

T = 2048
NT = 16
D = 1024
DFF = 2816
NE = 8
ALPHA = 4 ** 0.25
LAM_INIT = 0.8 - 0.6 * math.exp(-0.3 * 0)
EVEN_IN = 3104
ODD_IN = 416
FGROUPS = [(0, 4), (4, 8), (8, 12), (12, 16), (16, 19), (19, 22)]


class Sem:
    _n = 0

    def __init__(self, h):
        self.h = h
        self.v = 0
        Sem._n += 1
        self.uid = Sem._n


class KB:
    ENGS = ["sync", "scalar", "vector", "gpsimd", "tensor"]

    def __init__(self, nc, es):
        self.nc = nc
        self.es = es
        self.q = {e: [] for e in self.ENGS}
        self.waited = {e: {} for e in self.ENGS}
        self.esem = {e: self.new_sem("pg_" + e) for e in ["scalar", "vector", "gpsimd", "tensor"]}
        self.nsem = 0

    def new_sem(self, name):
        return Sem(self.es.enter_context(self.nc.semaphore(name)))

    def sbuf(self, name, shape, dt):
        return self.es.enter_context(self.nc.sbuf_tensor(name, shape, dt))

    def wait(self, eng, tok):
        if tok is None:
            return
        sem, val = tok
        key = sem.uid
        if self.waited[eng].get(key, 0) >= val:
            return
        self.waited[eng][key] = val
        getattr(self.nc, eng).wait_ge(sem.h, val)

    def op(self, eng, fn, waits=(), sig=False):
        for w in waits:
            self.wait(eng, w)
        ins = fn(getattr(self.nc, eng))
        if sig:
            s = self.esem[eng]
            s.v += 1
            ins.then_inc(s.h, 1)
            return (s, s.v)
        return None

    def dma(self, eng, out, in_, sem, waits=()):
        for w in waits:
            self.wait(eng, w)
        sem.v += 16
        getattr(self.nc, eng).dma_start(out=out, in_=in_).then_inc(sem.h, 16)
        return (sem, sem.v)


class PSBanks:
    def __init__(self, banks):
        self.banks = banks
        self.free = [[] for _ in banks]
        self.i = 0

    def get(self):
        b = self.i % len(self.banks)
        self.i += 1
        w = self.free[b]
        self.free[b] = []
        return b, self.banks[b], w

    def rel(self, b, tok):
        if tok is not None:
            self.free[b].append(tok)


class Ring:
    def __init__(self, bufs):
        self.bufs = bufs
        self.free = [[] for _ in bufs]
        self.i = 0

    def get(self):
        b = self.i % len(self.bufs)
        self.i += 1
        w = self.free[b]
        self.free[b] = []
        return b, self.bufs[b], w

    def rel(self, b, tok):
        if tok is not None:
            self.free[b].append(tok)


def _bucket(rel):
    half = 16
    max_exact = 8
    bucket = np.where(rel > 0, half, 0).astype(np.int32)
    n = np.abs(rel)
    n_large = max_exact + (np.log(np.maximum(n, max_exact).astype(np.float32) / max_exact)
                           / math.log(128 / max_exact) * (half - max_exact)).astype(np.int32)
    n_large = np.minimum(n_large, half - 1)
    return bucket + np.where(n < max_exact, n, n_large)


def host_consts():
    j = np.arange(128)[:, None]
    i = np.arange(128)[None, :]
    same = (j // 64) == (i // 64)
    c = {}
    triF = (same & (j <= i)).astype(np.float32)
    triB = (same & (j >= i)).astype(np.float32)
    cind = ((np.arange(128)[:, None] // 64) == np.arange(2)[None, :]).astype(np.float32)
    c["RF"] = np.concatenate([triF, cind], axis=1)
    c["RB"] = np.concatenate([triB, cind], axis=1)
    c["SU"] = (same & (j > i)).astype(np.float32)
    c["SL"] = (same & (j < i)).astype(np.float32)
    c["ident"] = np.eye(128, dtype=np.float32)
    half = 16
    inv = (10000.0 ** (-np.arange(half, dtype=np.float32) / half)).astype(np.float32)
    ang = np.arange(T, dtype=np.float32)[:, None] * inv[None, :]
    cos = np.cos(ang).astype(np.float32).T
    sin = np.sin(ang).astype(np.float32).T
    cosF = np.ones((96, T), np.float32)
    sinF = np.zeros((96, T), np.float32)
    cosF[64:80] = cos
    cosF[80:96] = cos
    sinF[64:80] = -sin
    sinF[80:96] = sin
    c["cosF"] = cosF
    c["sinF"] = sinF
    return c


def build(stage=99, dbg=None):
    import os
    nc = bass.Bass("TRN2", target_bir_lowering=False)
    es = ExitStack()
    with es:
        kb = KB(nc, es)

        def din(name, shape, dt=F32):
            return nc.dram_tensor(name, list(shape), dt, kind="ExternalInput").ap()

        def dscr(name, shape, dt):
            return nc.dram_tensor(name, list(shape), dt, kind="Internal").ap()

        IN_SHAPES = {
            "x_in": ("x", [T, D]),
            "w_in0": ("even_w_in", [D, EVEN_IN]),
            "gate_bd": ("gate_bd", [32, 512]),
            "gate_bias": ("gate_bias", [1, 512]),
            "gla_gain": ("gla_gain", [1, 128]),
            "diff_lam": ("diff_lambda", [1, 256]),
            "diff_gain": ("diff_gain", [1, 128]),
            "w_out0": ("even_w_out", [D, D]),
            "ffn_wg": ("ffn_w_gate", [D, DFF]),
            "ffn_wu": ("ffn_w_up", [D, DFF]),
            "ffn_wd": ("ffn_w_down", [DFF, D]),
            "w_in1": ("odd_w_in", [D, ODD_IN]),
            "w_in1_rot": ("odd_w_in_rot", [D, 32]),
            "qn_gain": ("mla_q_gain", [1, 256]),
            "kvn_gain": ("mla_kv_gain", [1, 128]),
            "w_uq": ("mla_w_uq", [256, 1536]),
            "w_uq_rot": ("mla_w_uq_rot", [256, 16 * 32]),
            "w_ukv": ("mla_w_ukv", [128, 2048]),
            "w_out1": ("odd_w_out", [D, D]),
            "router_wT": ("router_wT", [1, NE * D]),
            "moe_wg": ("moe_w_gate", [NE, D, DFF]),
            "moe_wu": ("moe_w_up", [NE, D, DFF]),
            "moe_wd": ("moe_w_down", [NE, DFF, D]),
            "ln_g": ("ln_gain", [4, D]),
            "ln_b": ("ln_bias", [4, D]),
            "toe_in": ("toe", [128, 4 * 6 * 512]),
            "far_in": ("far", [1, 8]),
            "c_RF": ("c_RF", [128, 130]),
            "c_RB": ("c_RB", [128, 130]),
            "c_SU": ("c_SU", [128, 128]),
            "c_SL": ("c_SL", [128, 128]),
            "c_ident": ("c_ident", [128, 128]),
            "c_cosF": ("c_cosF", [96, T]),
            "c_sinF": ("c_sinF", [96, T]),
        }
        in_cache = {}
        used_inputs = []

        def I(var):
            if var not in in_cache:
                name, shp = IN_SHAPES[var]
                in_cache[var] = nc.dram_tensor(name, list(shp), F32, kind="ExternalInput").ap()
                used_inputs.append(name)
            return in_cache[var]

        nc_used_inputs = used_inputs
        y_out = nc.dram_tensor("y", [T, D], F32, kind="ExternalOutput").ap()
        dbg_out = None
        if dbg is not None:
            dbg_out = nc.dram_tensor("dbg", list(dbg[0]), dbg[1], kind="ExternalOutput").ap()

        s_qgT = dscr("s_qgT", [256, T], BF16)
        s_kgT = dscr("s_kgT", [256, T], BF16)
        s_kg = dscr("s_kg", [T, 256], BF16)
        s_vg = dscr("s_vg", [T, 512], BF16)
        s_gg = dscr("s_gg", [T, 512], BF16)
        s_alrT = dscr("s_alrT", [32, T], BF16)
        s_qdT = dscr("s_qdT", [512, T], BF16)
        s_kdT = dscr("s_kdT", [512, T], BF16)
        s_vd = dscr("s_vd", [T, 512], BF16)
        s_cat = dscr("s_cat", [T, D], BF16)
        s_x1 = dscr("s_x1", [T, D], F32)
        s_x2 = dscr("s_x2", [T, D], F32)
        s_x3 = dscr("s_x3", [T, D], F32)
        s_attnT = dscr("s_attnT", [D, T], BF16)

        ident_b = kb.sbuf("ident_b", [128, 128], BF16)
        ident_f = kb.sbuf("ident_f", [128, 128], F32)
        ones_f = kb.sbuf("ones_f", [128, 128], F32)
        ones_b = kb.sbuf("ones_b", [128, 128], BF16)
        XA = kb.sbuf("XA", [128, 8, T], BF16)
        eps5 = kb.sbuf("eps5", [128, 1], F32)
        eps6 = kb.sbuf("eps6", [128, 1], F32)
        one1 = kb.sbuf("one1", [128, 1], F32)
        ln_stats = kb.sbuf("ln_stats", [128, NT, 2, 6], F32)
        ln_mv = kb.sbuf("ln_mv", [128, NT, 2], F32)
        ln_sc = kb.sbuf("ln_sc", [128, NT, 4], F32)
        bar_a = kb.sbuf("bar_a", [128, 8], F32)
        bar_v = kb.sbuf("bar_v", [128, 8], F32)
        bar_g = kb.sbuf("bar_g", [128, 8], F32)
        pds = [es.enter_context(nc.psum_tensor(f"pd{i}", [128, 1024], F32)) for i in range(4)]
        banks = [pds[i // 2][:, (i % 2) * 512:(i % 2 + 1) * 512] for i in range(8)]
        PSB = PSBanks(banks)

        PSB_T = PSBanks(banks[0:4])
        csem = kb.new_sem("csem")
        st_sem = kb.new_sem("st_sem")

        csem_g = kb.new_sem("csem_g")
        ctok0 = kb.dma("gpsimd", ident_b[:], I("c_ident")[:, :], csem_g)
        ctok = kb.dma("sync", ident_f[:], I("c_ident")[:, :], csem)
        kb.op("vector", lambda e: e.memset(ones_f[:], 1.0))
        kb.op("vector", lambda e: e.memset(ones_b[:], 1.0))
        kb.op("vector", lambda e: e.memset(eps5[:], 1e-5))
        kb.op("vector", lambda e: e.memset(one1[:], 1.0))
        ctok2 = kb.op("vector", lambda e: e.memset(eps6[:], 1e-6), sig=True)
        CW = [ctok0, ctok, ctok2]

        store_sems = [st_sem]

        def new_store_sem(name):
            s_ = kb.new_sem(name)
            store_sems.append(s_)
            return s_

        def store(out, in_, waits, sem=None):
            return kb.dma("sync", out, in_, sem if sem is not None else st_sem, waits=waits)

        def barrier():
            toks = [
                kb.op("scalar", lambda e: e.memzero(bar_a[:, 0:4]), sig=True),
                kb.op("vector", lambda e: e.memset(bar_v[:, 0:4], 0.0), sig=True),
                kb.op("gpsimd", lambda e: e.memset(bar_g[:, 0:4], 0.0), sig=True),
            ]
            for s_ in store_sems:
                if s_.v:
                    toks.append((s_, s_.v))
            for e in KB.ENGS:
                for tk in toks:
                    kb.wait(e, tk)

        evac_flip = [0]

        def evac(out, in_, waits, scale=None, func=None, eng=None):
            if func is not None:
                return kb.op("scalar", lambda e: e.activation(out=out, in_=in_, func=func,
                                                               scale=(1.0 if scale is None else scale)),
                             waits=waits, sig=True)
            if eng is None:
                evac_flip[0] ^= 1
                eng = "scalar" if evac_flip[0] else "vector"
            if eng == "scalar":
                if scale is None:
                    return kb.op("scalar", lambda e: e.copy(out=out, in_=in_), waits=waits, sig=True)
                return kb.op("scalar", lambda e: e.mul(out=out, in_=in_, mul=scale), waits=waits, sig=True)
            if scale is None:
                return kb.op("vector", lambda e: e.tensor_copy(out=out, in_=in_), waits=waits, sig=True)
            return kb.op("vector", lambda e: e.tensor_scalar(out=out, in0=in_, scalar1=scale, scalar2=None,
                                                              op0=ALU.mult), waits=waits, sig=True)

        def to_fm(src_b, t, dstT, nchunk, src_waits):
            toks = []
            for c0 in range(0, nchunk, 8):
                n = min(8, nchunk - c0)
                b, bank, w = PSB_T.get()
                pb = bank[:].bitcast(BF16)
                mt = None
                for c in range(n):
                    mt = kb.op("tensor", lambda e, c=c: e.transpose(
                        out=pb[:, c * 128:(c + 1) * 128], in_=src_b[:, (c0 + c) * 128:(c0 + c + 1) * 128],
                        identity=ident_b[:]), waits=list(w) + list(src_waits) + CW, sig=(c == n - 1))
                tk = evac(dstT[:, c0:c0 + n, t * 128:(t + 1) * 128],
                          pb[:, 0:n * 128].rearrange("p (c k) -> p c k", k=128), [mt])
                PSB_T.rel(b, tk)
                toks.append(tk)
            return toks

        def layer_norm_tile(t, y, gB, bB, out_f, waits):
            kb.op("vector", lambda e: e.bn_stats(out=ln_stats[:, t, 0, :], in_=y[:, 0:512]), waits=waits)
            tb = kb.op("vector", lambda e: e.bn_stats(out=ln_stats[:, t, 1, :], in_=y[:, 512:1024]), sig=True)
            tc_ = kb.op("vector", lambda e: e.bn_aggr(out=ln_mv[:, t, :],
                                                      in_=ln_stats[:, t, :, :].rearrange("p a s -> p (a s)")),
                        waits=[tb], sig=True)
            td = kb.op("scalar", lambda e: e.activation(out=ln_sc[:, t, 0:1], in_=ln_mv[:, t, 1:2], func=AF.Ln,
                                                        bias=eps5[:, 0:1]), waits=[tc_] + CW, sig=True)
            te = kb.op("scalar", lambda e: e.activation(out=ln_sc[:, t, 2:3], in_=ln_sc[:, t, 0:1], func=AF.Exp,
                                                        scale=-0.5), waits=[td], sig=True)
            tf = kb.op("vector", lambda e: e.scalar_tensor_tensor(out=ln_sc[:, t, 3:4], in0=ln_mv[:, t, 0:1],
                                                                   scalar=-1.0, in1=ln_sc[:, t, 2:3],
                                                                   op0=ALU.mult, op1=ALU.mult),
                       waits=[te], sig=True)
            tg = kb.op("scalar", lambda e: e.activation(out=out_f, in_=y, func=AF.Identity, bias=ln_sc[:, t, 3:4],
                                                        scale=ln_sc[:, t, 2:3]), waits=[tf], sig=True)
            th = kb.op("vector", lambda e: e.tensor_tensor(out=out_f, in0=out_f, in1=gB, op=ALU.mult),
                       waits=[tg], sig=True)
            return kb.op("vector", lambda e: e.tensor_tensor(out=out_f, in0=out_f, in1=bB, op=ALU.add),
                         waits=[th], sig=True)

        def load_ln_params(pes, li):
            sem = kb.new_sem(f"lnp{li}")
            gB = pes.enter_context(nc.sbuf_tensor(f"lngB{li}", [128, D], F32))
            bB = pes.enter_context(nc.sbuf_tensor(f"lnbB{li}", [128, D], F32))
            kb.dma("sync", gB[:], I("ln_g")[li, :].partition_broadcast(128), sem)
            tk = kb.dma("sync", bB[:], I("ln_b")[li, :].partition_broadcast(128), sem)
            return gB, bB, tk

        class LNRings:
            def __init__(self, pes, tag):
                mk = lambda nm, shp, dt, n: [pes.enter_context(nc.sbuf_tensor(f"{nm}{tag}{i}", shp, dt))
                                             for i in range(n)]
                self.xres = Ring(mk("lxr", [128, D], F32, 2))
                self.xres_sems = [kb.new_sem(f"lxrs{tag}{i}") for i in range(2)]
                self.y = Ring(mk("lyy", [128, D], F32, 2))
                self.out = Ring(mk("lout", [128, D], F32, 2))
                self.out_sems = [new_store_sem(f"louts{tag}{i}") for i in range(2)]
                self.b16 = Ring(mk("lb16", [128, D], BF16, 2))

        def resid_ln_tile(t, halves, xres_dram, gB, bB, gbtok, out_dram, out_XT, R):
            xb_, xres, xw = R.xres.get()
            lt = kb.dma("sync", xres[:], xres_dram[t * 128:(t + 1) * 128, :], R.xres_sems[xb_], waits=xw)
            yb_, y, yw = R.y.get()
            toks = []
            for dh, (src, sw) in enumerate(halves):
                tk = kb.op("vector", lambda e, dh=dh, src=src: e.scalar_tensor_tensor(
                    out=y[:, dh * 512:(dh + 1) * 512], in0=xres[:, dh * 512:(dh + 1) * 512], scalar=ALPHA, in1=src,
                    op0=ALU.mult, op1=ALU.add), waits=[lt] + list(sw) + list(yw), sig=True)
                toks.append(tk)
            R.xres.rel(xb_, toks[-1])
            ob_, o, ow = R.out.get()
            lt2 = layer_norm_tile(t, y[:], gB[:], bB[:], o[:], [toks[-1], gbtok] + list(ow))
            R.y.rel(yb_, lt2)
            stok = store(out_dram[t * 128:(t + 1) * 128, :], o[:], [lt2], R.out_sems[ob_])
            R.out.rel(ob_, stok)
            if out_XT is not None:
                bb_, ob16, bw = R.b16.get()
                ct = kb.op("scalar", lambda e: e.copy(out=ob16[:], in_=o[:]), waits=[lt2] + list(bw), sig=True)
                R.out.rel(ob_, ct)
                tks = to_fm(ob16, t, out_XT, 8, [ct])
                R.b16.rel(bb_, tks[-1])
            return toks

        def resid_ln_all(get_halves, ydst, xres_dram, gB, bB, gbtok, out_dram, out_XT, R, rel_cb=None):
            p1 = None
            for t in range(NT):
                halves = get_halves(t)
                xb_, xres, xw = R.xres.get()
                lt = kb.dma("sync", xres[:], xres_dram[t * 128:(t + 1) * 128, :], R.xres_sems[xb_], waits=xw)
                toks = []
                for dh, (src, sw) in enumerate(halves):
                    tk = kb.op("vector", lambda e, dh=dh, src=src: e.scalar_tensor_tensor(
                        out=ydst[:, t, dh * 512:(dh + 1) * 512], in0=xres[:, dh * 512:(dh + 1) * 512], scalar=ALPHA,
                        in1=src, op0=ALU.mult, op1=ALU.add), waits=[lt] + list(sw), sig=True)
                    toks.append(tk)
                R.xres.rel(xb_, toks[-1])
                if rel_cb is not None:
                    rel_cb(t, toks)
                kb.op("vector", lambda e: e.bn_stats(out=ln_stats[:, t, 0, :], in_=ydst[:, t, 0:512]), waits=toks)
                tb = kb.op("vector", lambda e: e.bn_stats(out=ln_stats[:, t, 1, :], in_=ydst[:, t, 512:1024]), sig=True)
                p1 = kb.op("vector", lambda e: e.bn_aggr(out=ln_mv[:, t, :],
                                                         in_=ln_stats[:, t, :, :].rearrange("p a s -> p (a s)")),
                           waits=[tb], sig=True)
            td = kb.op("scalar", lambda e: e.activation(out=ln_sc[:, :, 0], in_=ln_mv[:, :, 1], func=AF.Ln,
                                                        bias=eps5[:, 0:1]), waits=[p1] + CW, sig=True)
            te = kb.op("scalar", lambda e: e.activation(out=ln_sc[:, :, 2], in_=ln_sc[:, :, 0], func=AF.Exp,
                                                        scale=-0.5), waits=[td], sig=True)
            tf = kb.op("vector", lambda e: e.scalar_tensor_tensor(out=ln_sc[:, :, 3], in0=ln_mv[:, :, 0], scalar=-1.0,
                                                                   in1=ln_sc[:, :, 2], op0=ALU.mult, op1=ALU.mult),
                       waits=[te], sig=True)
            for t in range(NT):
                ob_, o, ow = R.out.get()
                tg = kb.op("scalar", lambda e: e.activation(out=o[:], in_=ydst[:, t, :], func=AF.Identity,
                                                            bias=ln_sc[:, t, 3:4], scale=ln_sc[:, t, 2:3]),
                           waits=[tf] + list(ow), sig=True)
                th = kb.op("vector", lambda e: e.tensor_tensor(out=o[:], in0=o[:], in1=gB[:], op=ALU.mult),
                           waits=[tg, gbtok], sig=True)
                ti = kb.op("vector", lambda e: e.tensor_tensor(out=o[:], in0=o[:], in1=bB[:], op=ALU.add),
                           waits=[th], sig=True)
                stok = store(out_dram[t * 128:(t + 1) * 128, :], o[:], [ti], R.out_sems[ob_])
                R.out.rel(ob_, stok)
                if out_XT is not None:
                    bb_, ob16, bw = R.b16.get()
                    ct = kb.op("scalar", lambda e: e.copy(out=ob16[:], in_=o[:]), waits=[ti] + list(bw), sig=True)
                    R.out.rel(ob_, ct)
                    tks = to_fm(ob16, t, out_XT, 8, [ct])
                    R.b16.rel(bb_, tks[-1])

        def finish():
            for s_ in store_sems:
                if s_.v:
                    kb.wait("sync", (s_, s_.v))

        gla_es = ExitStack()
        qgT = gla_es.enter_context(nc.sbuf_tensor("qgT", [128, 2, T], BF16))
        kgT = gla_es.enter_context(nc.sbuf_tensor("kgT", [128, 2, T], BF16))
        kgm = gla_es.enter_context(nc.sbuf_tensor("kgm", [128, NT, 256], BF16))
        vg = gla_es.enter_context(nc.sbuf_tensor("vg", [128, NT, 512], BF16))
        alrT = gla_es.enter_context(nc.sbuf_tensor("alrT", [32, T], BF16))
        wA_es = ExitStack()
        wA = wA_es.enter_context(nc.sbuf_tensor("wA", [128, 8, EVEN_IN], BF16))
        with ExitStack() as pes:
            xb_bufs = [pes.enter_context(nc.sbuf_tensor(f"xb{i}", [128, D], BF16)) for i in range(2)]
            xb_sems = [kb.new_sem(f"xbs{i}") for i in range(2)]
            ring = Ring(xb_bufs)
            for t in range(NT):
                b, xb, w = ring.get()
                lt = kb.dma("gpsimd", xb[:], I("x_in")[t * 128:(t + 1) * 128, :], xb_sems[b], waits=w)
                toks = to_fm(xb, t, XA, 8, [lt])
                ring.rel(b, toks[-1])
            wsem = kb.new_sem("wAsem")
            wv = I("w_in0").rearrange("(c p) n -> p c n", p=128)
            wtok = None
            for c in range(8):
                wtok = kb.dma("gpsimd", wA[:, c, :], wv[:, c, :], wsem)
            barrier()

        if stage == 0:
            store(dbg_out.rearrange("(c p) t -> p c t", p=128), XA[:], [])
            finish()
            nc.used_inputs = list(used_inputs)
            return nc

        with ExitStack() as pes:
            stg = Ring([pes.enter_context(nc.sbuf_tensor(f"stgA{i}", [128, 512], BF16)) for i in range(4)])
            stg_sems = [new_store_sem(f"stgAs{i}") for i in range(4)]
            fm_list = []
            for c in range(2):
                fm_list.append((c * 128, 128, None, 0.125, qgT[:, c, :]))
            for c in range(2):
                fm_list.append((256 + c * 128, 128, None, None, kgT[:, c, :]))
            fm_list.append((1536, 32, None, None, alrT[0:32, :]))
            for c in range(4):
                fm_list.append((1568 + c * 128, 128, s_qdT[c * 128:(c + 1) * 128, :], 0.125, None))
            for c in range(4):
                fm_list.append((2080 + c * 128, 128, s_kdT[c * 128:(c + 1) * 128, :], None, None))
            for (c0, M, dst, scale, res) in fm_list:
                for tt in range(4):
                    b, bank, w = PSB.get()
                    mt = None
                    for kc in range(8):
                        mt = kb.op("tensor", lambda e, kc=kc: e.matmul(
                            bank[0:M, :], lhsT=wA[:, kc, c0:c0 + M], rhs=XA[:, kc, tt * 512:(tt + 1) * 512],
                            start=(kc == 0), stop=(kc == 7)), waits=list(w) + [wtok], sig=(kc == 7))
                    if res is not None:
                        tk = evac(res[:, tt * 512:(tt + 1) * 512], bank[0:M, :], [mt], scale=scale)
                        PSB.rel(b, tk)
                        continue
                    sb, st, sw = stg.get()
                    tk = evac(st[0:M, :], bank[0:M, :], [mt] + list(sw), scale=scale)
                    PSB.rel(b, tk)
                    stok = store(dst[:, tt * 512:(tt + 1) * 512], st[0:M, :], [tk], stg_sems[sb])
                    stg.rel(sb, stok)
            tm_list = [(256, 256, None, None, kgm), (512, 512, None, None, vg), (1024, 512, s_gg, AF.Silu, None),
                       (2592, 512, s_vd, None, None)]
            for (c0, W, dst, func, res) in tm_list:
                for t in range(NT):
                    b, bank, w = PSB.get()
                    mt = None
                    for kc in range(8):
                        mt = kb.op("tensor", lambda e, kc=kc: e.matmul(
                            bank[:, 0:W], lhsT=XA[:, kc, t * 128:(t + 1) * 128], rhs=wA[:, kc, c0:c0 + W],
                            start=(kc == 0), stop=(kc == 7)), waits=list(w) + [wtok], sig=(kc == 7))
                    if res is not None:
                        tk = evac(res[:, t, :], bank[:, 0:W], [mt], func=func)
                        PSB.rel(b, tk)
                        continue
                    sb, st, sw = stg.get()
                    tk = evac(st[:, 0:W], bank[:, 0:W], [mt] + list(sw), func=func)
                    PSB.rel(b, tk)
                    stok = store(dst[t * 128:(t + 1) * 128, :], st[:, 0:W], [tk], stg_sems[sb])
                    stg.rel(sb, stok)
            barrier()
        wA_es.close()

        if stage == 1:
            with nc.sbuf_tensor("dbgt", [128, 16, 256], BF16) as dt_:
                sm = kb.new_sem("dbgs")
                tk = kb.dma("sync", dt_[:], s_kg.rearrange("(t p) n -> p t n", p=128), sm)
                store(dbg_out.rearrange("(t p) n -> p t n", p=128), dt_[:], [tk])
                finish()
            nc.used_inputs = list(used_inputs)
            return nc

        def dump_dram(view, shape3, dt):
            with nc.sbuf_tensor("dbgt", list(shape3), dt) as dt_:
                sm = kb.new_sem("dbgs")
                tk = kb.dma("sync", dt_[:], view, sm)
                return dt_, tk

        if True:
            with ExitStack() as pes:
                sb = lambda nm, shp, dt: pes.enter_context(nc.sbuf_tensor(nm, shp, dt))
                gbd = sb("gbd", [32, 512], BF16)
                gbs = sb("gbs", [1, 512], BF16)
                RF = sb("RF", [128, 130], F32)
                RB = sb("RB", [128, 130], F32)
                SU = sb("SU", [128, 128], F32)
                SL = sb("SL", [128, 128], F32)
                mFB = sb("mFB", [128, 256], F32)
                gainG = sb("gainG", [128, 128], F32)
                qf, kf, qb, kbb = XA[:, 0:2, :], XA[:, 2:4, :], XA[:, 4:6, :], XA[:, 6:8, :]
                ke = sb("ke", [128, NT, 512], BF16)
                dec = sb("dec", [128, 2, 2, 32], F32)
                S_all = sb("S_all", [128, 2, 2, 32, 128], BF16)
                S32 = sb("S32", [128, 2, 2, 2, 128], F32)
                catg = sb("catg", [128, NT, 512], BF16)
                ssg = sb("ssg", [128, NT, 4], F32)
                rsg = sb("rsg", [128, NT, 8], F32)
                e1r = Ring([sb(f"e1b{i}", [128, 512], F32) for i in range(2)])
                lpr = Ring([sb(f"lpb{i}", [128, 512], F32) for i in range(2)])
                gker = Ring([sb(f"gke{i}", [128, 512], F32) for i in range(2)])
                gtr = Ring([sb(f"gts{i}", [128, 2, 2, 2, 128], F32) for i in range(2)])
                amr = Ring([sb(f"am{i}", [128, 256], BF16) for i in range(8)])
                osr = Ring([sb(f"osb{i}", [128, 512], F32) for i in range(2)])
                tnr = Ring([sb(f"tnb{i}", [128, 512], F32) for i in range(2)])
                sgr_ = Ring([sb(f"sgb{i}", [128, 512], BF16) for i in range(2)])
                sg_sems = [kb.new_sem(f"sgs{i}") for i in range(2)]
                jg = sb("jg", [128, 4, 128], F32)
                jg_tok = [None] * 4
                ls = kb.new_sem("gla_s")
                lg = kb.new_sem("gla_g")
                kb.dma("sync", RF[:], I("c_RF")[:, :], ls)
                kb.dma("sync", RB[:], I("c_RB")[:, :], ls)
                kb.dma("sync", SU[:], I("c_SU")[:, :], ls)
                kb.dma("sync", SL[:], I("c_SL")[:, :], ls)
                kb.dma("sync", mFB[:, 0:128], I("c_RF")[:, 0:128], ls)
                kb.dma("sync", mFB[:, 128:256], I("c_RB")[:, 0:128], ls)
                ltok = kb.dma("sync", gainG[:], I("gla_gain")[0, :].partition_broadcast(128), ls)
                kb.dma("gpsimd", gbd[:], I("gate_bd")[:, :], lg)
                gtok = kb.dma("gpsimd", gbs[:], I("gate_bias")[:, :], lg)
                LW = [ltok, gtok]
                tz0 = kb.op("vector", lambda e: e.memset(S32[:].rearrange("p a b c d -> p (a b c d)"), 0.0), sig=True)
                tz1 = kb.op("gpsimd", lambda e: e.memset(S_all[:, 0, :, 0, :], 0.0), sig=True)
                tz2 = kb.op("gpsimd", lambda e: e.memset(S_all[:, 1, :, 31, :], 0.0), sig=True)
                PS1 = PSBanks(banks[0:8])
                p1_toks = []
                def gp_stage1(t):
                        sl = slice(t * 128, (t + 1) * 128)
                        bz, bankz, wz = PS1.get()
                        kb.op("tensor", lambda e: e.matmul(bankz[:, :], lhsT=alrT[0:32, sl], rhs=gbd[0:32, :], start=True,
                                                           stop=False), waits=list(wz) + LW + CW)
                        mz = kb.op("tensor", lambda e: e.matmul(bankz[:, :], lhsT=ones_b[0:1, 0:128], rhs=gbs[0:1, :],
                                                                start=False, stop=True), sig=True)
                        ei, e1, ew = e1r.get()
                        te1 = kb.op("scalar", lambda e: e.activation(out=e1[:], in_=bankz[:, :], func=AF.Exp, scale=-1.0),
                                    waits=[mz] + list(ew), sig=True)
                        PS1.rel(bz, te1)
                        li_, lp, lw = lpr.get()
                        tlp = kb.op("scalar", lambda e: e.activation(out=lp[:], in_=e1[:], func=AF.Ln, bias=one1[:, 0:1]),
                                    waits=[te1] + list(lw) + CW, sig=True)
                        e1r.rel(ei, tlp)
                        return (li_, lp, tlp)

                def gp_stage2(t, ctx):
                        li_, lp, tlp = ctx
                        sl = slice(t * 128, (t + 1) * 128)
                        bf_, bankF, wf = PS1.get()
                        bb_, bankB, wb = PS1.get()
                        be_, bankE, we = PS1.get()
                        mF = mB = mE = None
                        for p in range(2):
                            mF = kb.op("tensor", lambda e: e.matmul(bankF[:, p * 130:(p + 1) * 130],
                                                                    lhsT=lp[:, p * 128:(p + 1) * 128], rhs=RF[:, :],
                                                                    start=True, stop=True), waits=[tlp] + list(wf), sig=(p == 1))
                        for p in range(2):
                            mB = kb.op("tensor", lambda e: e.matmul(bankB[:, p * 130:(p + 1) * 130],
                                                                    lhsT=lp[:, 256 + p * 128:256 + (p + 1) * 128], rhs=RB[:, :],
                                                                    start=True, stop=True), waits=list(wb), sig=(p == 1))
                        kb.op("tensor", lambda e: e.matmul(bankE[:, 0:256], lhsT=SU[:, :], rhs=lp[:, 0:256], start=True,
                                                           stop=True), waits=list(we))
                        mE = kb.op("tensor", lambda e: e.matmul(bankE[:, 256:512], lhsT=SL[:, :], rhs=lp[:, 256:512],
                                                                start=True, stop=True), sig=True)
                        lpr.rel(li_, mE)
                        gi_, gts, gw = gtr.get()
                        vF = bankF[:, 0:260].rearrange("p (a c) -> p a c", c=130)
                        vB = bankB[:, 0:260].rearrange("p (a c) -> p a c", c=130)
                        kb.op("scalar", lambda e: e.activation(out=gts[:, 0, 0, :, :], in_=vF[:, :, 0:128], func=AF.Exp,
                                                               scale=-1.0 / 16), waits=[mF] + list(gw))
                        kb.op("scalar", lambda e: e.activation(out=gts[:, 0, 1, :, :], in_=vF[:, :, 0:128], func=AF.Exp,
                                                               scale=1.0 / 16))
                        tdF = kb.op("scalar", lambda e: e.activation(out=dec[:, 0, :, 2 * t:2 * t + 2], in_=vF[:, :, 128:130],
                                                                     func=AF.Exp, scale=-1.0 / 16), sig=True)
                        PS1.rel(bf_, tdF)
                        kb.op("scalar", lambda e: e.activation(out=gts[:, 1, 0, :, :], in_=vB[:, :, 0:128], func=AF.Exp,
                                                               scale=-1.0 / 16), waits=[mB])
                        kb.op("scalar", lambda e: e.activation(out=gts[:, 1, 1, :, :], in_=vB[:, :, 0:128], func=AF.Exp,
                                                               scale=1.0 / 16))
                        tdB = kb.op("scalar", lambda e: e.activation(out=dec[:, 1, :, 2 * t:2 * t + 2], in_=vB[:, :, 128:130],
                                                                     func=AF.Exp, scale=-1.0 / 16), sig=True)
                        PS1.rel(bb_, tdB)
                        ki_, gke, kw_ = gker.get()
                        tke = kb.op("scalar", lambda e: e.activation(out=gke[:], in_=bankE[:, :], func=AF.Exp,
                                                                     scale=-1.0 / 16), waits=[mE] + list(kw_), sig=True)
                        PS1.rel(be_, tke)
                        a1 = kb.op("vector", lambda e: e.tensor_tensor(out=qf[:, :, sl], in0=qgT[:, :, sl], in1=gts[:, 0, 0, :, :],
                                                                       op=ALU.mult), waits=[tdB] + LW, sig=True)
                        a2 = kb.op("vector", lambda e: e.tensor_tensor(out=qb[:, :, sl], in0=qgT[:, :, sl], in1=gts[:, 1, 0, :, :],
                                                                       op=ALU.mult), sig=True)
                        a3 = kb.op("vector", lambda e: e.tensor_tensor(out=ke[:, t, 0:256], in0=kgm[:, t, :], in1=gke[:, 0:256],
                                                                       op=ALU.mult), waits=[tke], sig=True)
                        g1 = kb.op("gpsimd", lambda e: e.tensor_tensor(out=kf[:, :, sl], in0=kgT[:, :, sl], in1=gts[:, 0, 1, :, :],
                                                                       op=ALU.mult), waits=[tdB] + LW, sig=True)
                        g2 = kb.op("gpsimd", lambda e: e.tensor_tensor(out=kbb[:, :, sl], in0=kgT[:, :, sl],
                                                                       in1=gts[:, 1, 1, :, :], op=ALU.mult), sig=True)
                        g3 = kb.op("gpsimd", lambda e: e.tensor_tensor(out=ke[:, t, 256:512], in0=kgm[:, t, :],
                                                                       in1=gke[:, 256:512], op=ALU.mult), waits=[tke], sig=True)
                        gtr.rel(gi_, a2)
                        gtr.rel(gi_, g2)
                        gker.rel(ki_, a3)
                        gker.rel(ki_, g3)
                        p1_toks = [a1, a2, a3, g1, g2, g3, tdF, tdB]
                        return [a1, a2, a3, g1, g2, g3, tdF, tdB]

                gp_ctx = {0: gp_stage1(0)}
                for t in range(NT):
                    if t + 1 < NT:
                        gp_ctx[t + 1] = gp_stage1(t + 1)
                    p1_toks = gp_stage2(t, gp_ctx[t])
                P1 = list(p1_toks)
                PS2 = PSBanks(banks[0:4])
                orders = [list(range(0, 31)), list(range(31, 0, -1))]
                prev_copy = [tz0, tz0]
                for step in range(31):
                    for d_ in range(2):
                        n = orders[d_][step]
                        t = n // 2
                        rows = slice((n % 2) * 64, (n % 2) * 64 + 64)
                        bk, bankK, wk = PS2.get()
                        mk = None
                        for p in range(2):
                            for hh in range(2):
                                h = p * 2 + hh
                                mk = kb.op("tensor", lambda e: e.matmul(
                                    bankK[hh * 64:(hh + 1) * 64, p * 128:(p + 1) * 128],
                                    lhsT=ke[rows, t, d_ * 256 + h * 64:d_ * 256 + (h + 1) * 64],
                                    rhs=vg[rows, t, h * 128:(h + 1) * 128], start=True, stop=True),
                                    waits=list(wk) + P1, sig=(h == 3))
                        cur, nxt = step % 2, (step + 1) % 2
                        ts_ = None
                        for p in range(2):
                            ts_ = kb.op("vector", lambda e: e.scalar_tensor_tensor(
                                out=S32[:, d_, nxt, p, :], in0=S32[:, d_, cur, p, :], scalar=dec[:, d_, p, n:n + 1],
                                in1=bankK[:, p * 128:(p + 1) * 128], op0=ALU.mult, op1=ALU.add),
                                waits=[mk, prev_copy[d_]] + P1, sig=True)
                        PS2.rel(bk, ts_)
                        dst_n = n + 1 if d_ == 0 else n - 1
                        prev_copy[d_] = kb.op("vector", lambda e: e.tensor_copy(out=S_all[:, d_, :, dst_n, :],
                                                                                in_=S32[:, d_, nxt, :, :]),
                                              waits=[ts_, tz1, tz2], sig=True)
                P1 = P1 + prev_copy
                PSA = PSBanks(banks[0:4])
                PSO2 = PSBanks(banks[4:8])
                tfin = None
                def g_stage1(t):
                    sl = slice(t * 128, (t + 1) * 128)
                    res = []
                    for h in range(4):
                        p, hh = h // 2, h % 2
                        r = slice(hh * 64, hh * 64 + 64)
                        ba, bankA, wa = PSA.get()
                        kb.op("tensor", lambda e: e.matmul(bankA[:, 0:128], lhsT=kf[r, p, sl], rhs=qf[r, p, sl], start=True,
                                                           stop=True), waits=list(wa) + P1)
                        ma = kb.op("tensor", lambda e: e.matmul(bankA[:, 128:256], lhsT=kbb[r, p, sl], rhs=qb[r, p, sl],
                                                                start=True, stop=True), sig=True)
                        ai, am, aw = amr.get()
                        tam = kb.op("vector", lambda e: e.tensor_tensor(out=am[:], in0=bankA[:, 0:256], in1=mFB[:],
                                                                        op=ALU.mult), waits=[ma] + list(aw) + LW, sig=True)
                        PSA.rel(ba, tam)
                        res.append((ai, am, tam))
                    return res

                g_ctx = {0: g_stage1(0)}
                for t in range(NT):
                    if t + 1 < NT:
                        g_ctx[t + 1] = g_stage1(t + 1)
                    sl = slice(t * 128, (t + 1) * 128)
                    si_, sgt, sw = sgr_.get()
                    lsg = kb.dma("sync", sgt[:], s_gg[t * 128:(t + 1) * 128, :], sg_sems[si_], waits=sw)
                    bo, banko, wo_ = PSO2.get()
                    mo = None
                    for h in range(4):
                        p, hh = h // 2, h % 2
                        r = slice(hh * 64, hh * 64 + 64)
                        ai, am, tam = g_ctx[t][h]
                        oh = banko[:, h * 128:(h + 1) * 128]
                        kb.op("tensor", lambda e: e.matmul(oh, lhsT=am[:, 0:128], rhs=vg[:, t, h * 128:(h + 1) * 128],
                                                           start=True, stop=False, skip_group_check=True),
                              waits=[tam] + (list(wo_) if h == 0 else []))
                        kb.op("tensor", lambda e: e.matmul(oh, lhsT=am[:, 128:256], rhs=vg[:, t, h * 128:(h + 1) * 128],
                                                           start=False, stop=False, skip_group_check=True))
                        for c in range(2):
                            cs = slice(t * 128 + c * 64, t * 128 + c * 64 + 64)
                            kb.op("tensor", lambda e: e.matmul(banko[c * 64:(c + 1) * 64, h * 128:(h + 1) * 128],
                                                               lhsT=qf[r, p, cs], rhs=S_all[r, 0, p, 2 * t + c, :],
                                                               start=False, stop=False, skip_group_check=True))
                        for c in range(2):
                            cs = slice(t * 128 + c * 64, t * 128 + c * 64 + 64)
                            mo = kb.op("tensor", lambda e: e.matmul(banko[c * 64:(c + 1) * 64, h * 128:(h + 1) * 128],
                                                                    lhsT=qb[r, p, cs], rhs=S_all[r, 1, p, 2 * t + c, :],
                                                                    start=False, stop=(c == 1), skip_group_check=True),
                                       sig=(c == 1))
                        amr.rel(ai, mo)
                    oi_, osb, ow = osr.get()
                    tcp = kb.op("scalar", lambda e: e.copy(out=osb[:], in_=banko[:, :]), waits=[mo] + list(ow), sig=True)
                    PSO2.rel(bo, tcp)
                    tss = None
                    for h in range(4):
                        tss = kb.op("vector", lambda e: e.scalar_tensor_tensor(
                            out=jg[:, h, :], in0=osb[:, h * 128:(h + 1) * 128], scalar=1.0, in1=osb[:, h * 128:(h + 1) * 128],
                            op0=ALU.mult, op1=ALU.mult, accum_out=ssg[:, t, h:h + 1]), waits=[tcp, jg_tok[h]], sig=True)
                        jg_tok[h] = tss
                    tln = kb.op("scalar", lambda e: e.activation(out=rsg[:, t, 0:4], in_=ssg[:, t, 0:4], func=AF.Ln,
                                                                 bias=eps6[:, 0:1], scale=1.0 / 128), waits=[tss] + CW,
                                sig=True)
                    tex = kb.op("scalar", lambda e: e.activation(out=rsg[:, t, 4:8], in_=rsg[:, t, 0:4], func=AF.Exp,
                                                                 scale=-0.5), waits=[tln], sig=True)
                    ni, tn_, nw = tnr.get()
                    tno = None
                    for h in range(4):
                        tno = kb.op("vector", lambda e: e.scalar_tensor_tensor(
                            out=tn_[:, h * 128:(h + 1) * 128], in0=osb[:, h * 128:(h + 1) * 128],
                            scalar=rsg[:, t, 4 + h:5 + h], in1=gainG[:], op0=ALU.mult, op1=ALU.mult),
                            waits=[tex] + list(nw) + LW, sig=True)
                    osr.rel(oi_, tno)
                    tfin = kb.op("gpsimd", lambda e: e.tensor_tensor(out=catg[:, t, :], in0=tn_[:], in1=sgt[:], op=ALU.mult),
                                 waits=[tno, lsg], sig=True)
                    tnr.rel(ni, tfin)
                    sgr_.rel(si_, tfin)
                store(s_cat.rearrange("(t p) n -> p t n", p=128)[:, :, 0:512], catg[:], [tfin])
                barrier()
        gla_es.close()

        with ExitStack() as pes:
            sb = lambda nm, shp, dt: pes.enter_context(nc.sbuf_tensor(nm, shp, dt))
            qd = sb("qd", [128, 4, T], BF16)
            kd = sb("kd", [128, 4, T], BF16)
            vaug = sb("vaugd", [128, NT, 4, 130], BF16)
            toe = sb("toe_sb", [128, 4, 6, 512], F32)
            far = sb("far_sb", [128, 8], F32)
            dl = sb("dl", [128, 256], F32)
            dlp = sb("dlp", [128, 128], F32)
            lam_s = sb("lam_s", [128, 8], F32)
            gainD = sb("gainD", [128, 128], F32)
            catd = sb("catd", [128, NT, 512], BF16)
            o0 = sb("o0", [128, 4, 128], F32)
            odr = Ring([sb(f"od{i}", [128, 4, 128], F32) for i in range(2)])
            junk = sb("junkd", [128, 4, 128], F32)
            junk_tok = [None] * 4
            rcs = sb("rcs", [128, 32, 16], F32)
            rstd_t = sb("rstd_t", [128, 16, 8], F32)
            ptr = Ring([sb(f"ptd{i}", [128, 512], BF16) for i in range(3)])
            tmpr = Ring([sb(f"tmpd{i}", [128, 512], F32) for i in range(2)])
            ls = kb.new_sem("dls")
            lg = kb.new_sem("dlg")
            kb.dma("sync", qd[:], s_qdT.rearrange("(c p) t -> p c t", p=128), ls)
            kb.dma("sync", kd[:], s_kdT.rearrange("(c p) t -> p c t", p=128), ls)
            for hh in range(4):
                kb.dma("sync", vaug[:, :, hh, 0:128],
                       s_vd.rearrange("(t p) n -> p t n", p=128)[:, :, hh * 128:(hh + 1) * 128], ls)
            kb.dma("sync", toe[:].rearrange("p a b c -> p (a b c)"), I("toe_in")[:, :], ls)
            kb.dma("sync", far[:], I("far_in")[0, :].partition_broadcast(128), ls)
            kb.dma("sync", dl[:], I("diff_lam")[0, :].partition_broadcast(128), ls)
            ldtok = kb.dma("sync", gainD[:], I("diff_gain")[0, :].partition_broadcast(128), ls)
            t_ones = kb.op("gpsimd", lambda e: e.memset(vaug[:, :, :, 128:129], 1.0), sig=True)
            t_g = kb.op("vector", lambda e: e.tensor_scalar(out=gainD[:], in0=gainD[:], scalar1=1.0 - LAM_INIT,
                                                            scalar2=None, op0=ALU.mult), waits=[ldtok], sig=True)
            t_a = kb.op("vector", lambda e: e.tensor_tensor(out=dlp[:, 0:64], in0=dl[:, 0:64], in1=dl[:, 64:128],
                                                            op=ALU.mult), waits=[ldtok], sig=True)
            t_b = kb.op("vector", lambda e: e.tensor_tensor(out=dlp[:, 64:128], in0=dl[:, 128:192], in1=dl[:, 192:256],
                                                            op=ALU.mult), sig=True)
            t_c = kb.op("vector", lambda e: e.tensor_reduce(out=lam_s[:, 0:2],
                                                            in_=dlp[:].rearrange("p (a d) -> p a d", a=2),
                                                            axis=mybir.AxisListType.X, op=ALU.add),
                        waits=[t_a, t_b], sig=True)
            t_d = kb.op("scalar", lambda e: e.activation(out=lam_s[:, 2:4], in_=lam_s[:, 0:2], func=AF.Exp),
                        waits=[t_c], sig=True)
            t_e = kb.op("vector", lambda e: e.tensor_tensor(out=lam_s[:, 4:5], in0=lam_s[:, 3:4], in1=lam_s[:, 2:3],
                                                            op=ALU.subtract), waits=[t_d], sig=True)
            t_lam = kb.op("vector", lambda e: e.tensor_scalar(out=lam_s[:, 5:6], in0=lam_s[:, 4:5], scalar1=-LAM_INIT,
                                                              scalar2=None, op0=ALU.add), waits=[t_e], sig=True)
            PS_S = PSBanks(banks[0:4])
            ACC = banks[4:8]
            acc_free = [[] for _ in range(4)]
            LOADW = [ldtok, t_ones]
            dctx = {"o0_tok": None, "tf": None}

            PS_D2 = PSBanks([pds[0], pds[1]])
            pt2d = Ring([sb(f"pt2d{i}", [128, 1024], BF16) for i in range(3)])
            tmp2r = Ring([sb(f"tmp2d{i}", [128, 1024], F32) for i in range(2)])

            def d_stage1(it):
                h, qt, m, kp = it
                b_, pd, w = PS_D2.get()
                mt = None
                for j in range(2):
                    kc = 2 * kp + j
                    mt = kb.op("tensor", lambda e: e.matmul(
                        pd[:, j * 512:(j + 1) * 512], lhsT=kd[m * 64:(m + 1) * 64, h, kc * 128:(kc + 1) * 128],
                        rhs=qd[m * 64:(m + 1) * 64, h, qt * 512:(qt + 1) * 512], start=True, stop=True),
                        waits=list(w) + LOADW, sig=(j == 1))
                pb_, PT, pw = pt2d.get()
                mrels = [2 * kp + j - 4 * qt for j in range(2)]
                near = [(-1 <= r <= 4) for r in mrels]
                if not any(near) and (mrels[0] > 0) == (mrels[1] > 0):
                    fi = h * 2 + (1 if mrels[0] > 0 else 0)
                    t2 = kb.op("scalar", lambda e: e.activation(out=PT[:], in_=pd[:, :], func=AF.Exp,
                                                                bias=far[:, fi:fi + 1]),
                               waits=[mt] + list(pw) + LOADW, sig=True)
                    PS_D2.rel(b_, t2)
                else:
                    tb_, tmp, tw = tmp2r.get()
                    t1 = None
                    for j in range(2):
                        if near[j]:
                            t1 = kb.op("vector", lambda e: e.tensor_tensor(
                                out=tmp[:, j * 512:(j + 1) * 512], in0=pd[:, j * 512:(j + 1) * 512],
                                in1=toe[:, h, mrels[j] + 1, :], op=ALU.add), waits=[mt] + list(tw) + LOADW, sig=True)
                        else:
                            fi = h * 2 + (1 if mrels[j] > 0 else 0)
                            t1 = kb.op("vector", lambda e: e.tensor_scalar(
                                out=tmp[:, j * 512:(j + 1) * 512], in0=pd[:, j * 512:(j + 1) * 512],
                                scalar1=far[:, fi:fi + 1], scalar2=None, op0=ALU.add),
                                waits=[mt] + list(tw) + LOADW, sig=True)
                    PS_D2.rel(b_, t1)
                    t2 = kb.op("scalar", lambda e: e.activation(out=PT[:], in_=tmp[:], func=AF.Exp),
                               waits=[t1] + list(pw), sig=True)
                    tmp2r.rel(tb_, t2)
                return (pb_, PT, t2)

            def d_stage2(it, ctx):
                h, qt, m, kp = it
                u = h * 4 + qt
                pb_, PT, t2 = ctx
                pv_last = None
                for j in range(2):
                    kc = 2 * kp + j
                    for qs in range(4):
                        w2 = [t2] + (acc_free[qs] if kc == 0 else [])
                        if kc == 0:
                            acc_free[qs] = []
                        pv_last = kb.op("tensor", lambda e: e.matmul(
                            ACC[qs][:, 0:129], lhsT=PT[:, j * 512 + qs * 128:j * 512 + (qs + 1) * 128],
                            rhs=vaug[:, kc, h, 0:129], start=(kc == 0), stop=(kc == NT - 1)), waits=w2,
                            sig=(qs == 3 and j == 1))
                pt2d.rel(pb_, pv_last)
                if kp != NT // 2 - 1:
                    return
                tr = None
                for qs in range(4):
                    tr = kb.op("vector", lambda e, qs=qs: e.reciprocal(out=rcs[:, u, m * 4 + qs:m * 4 + qs + 1],
                                                                        in_=ACC[qs][:, 128:129]),
                               waits=[pv_last], sig=(qs == 3))
                if m == 0:
                    tk = None
                    for qs in range(4):
                        tk = kb.op("vector", lambda e, qs=qs: e.tensor_scalar(
                            out=o0[:, qs, :], in0=ACC[qs][:, 0:128], scalar1=rcs[:, u, qs:qs + 1], scalar2=None,
                            op0=ALU.mult), waits=[tr, dctx["tf"]], sig=True)
                        acc_free[qs].append(tk)
                    dctx["o0_tok"] = tk
                else:
                    tl = kb.op("vector", lambda e: e.tensor_scalar(out=rcs[:, u, 8:12], in0=rcs[:, u, 4:8],
                                                                   scalar1=lam_s[:, 5:6], scalar2=None,
                                                                   op0=ALU.mult), waits=[tr, t_lam], sig=True)
                    ob_, od, ow = odr.get()
                    tod = None
                    for qs in range(4):
                        tod = kb.op("vector", lambda e, qs=qs: e.scalar_tensor_tensor(
                            out=od[:, qs, :], in0=ACC[qs][:, 0:128], scalar=rcs[:, u, 8 + qs:9 + qs],
                            in1=o0[:, qs, :], op0=ALU.mult, op1=ALU.add), waits=[tl, dctx["o0_tok"]] + list(ow),
                            sig=True)
                        acc_free[qs].append(tod)
                    tss = None
                    for qs in range(4):
                        tss = kb.op("vector", lambda e, qs=qs: e.scalar_tensor_tensor(
                            out=junk[:, qs, :], in0=od[:, qs, :], scalar=1.0, in1=od[:, qs, :], op0=ALU.mult,
                            op1=ALU.mult, accum_out=rcs[:, u, 12 + qs:13 + qs]), waits=[tod, junk_tok[qs]],
                            sig=True)
                        junk_tok[qs] = tss
                    tln = kb.op("scalar", lambda e: e.activation(out=rstd_t[:, u, 0:4], in_=rcs[:, u, 12:16],
                                                                 func=AF.Ln, bias=eps6[:, 0:1], scale=1.0 / 128),
                                waits=[tss] + CW, sig=True)
                    tex = kb.op("scalar", lambda e: e.activation(out=rstd_t[:, u, 4:8], in_=rstd_t[:, u, 0:4],
                                                                 func=AF.Exp, scale=-0.5), waits=[tln], sig=True)
                    tf_ = None
                    for qs in range(4):
                        tf_ = kb.op("vector", lambda e, qs=qs: e.scalar_tensor_tensor(
                            out=catd[:, qt * 4 + qs, h * 128:(h + 1) * 128], in0=od[:, qs, :],
                            scalar=rstd_t[:, u, 4 + qs:5 + qs], in1=gainD[:], op0=ALU.mult, op1=ALU.mult),
                            waits=[tex, t_g], sig=True)
                    odr.rel(ob_, tf_)
                    dctx["tf"] = tf_

            ditems = [(h, qt, m, kp) for h in range(4) for qt in range(4) for m in range(2) for kp in range(NT // 2)]
            prev = None
            for it in ditems:
                ctx = d_stage1(it)
                if prev is not None:
                    d_stage2(*prev)
                prev = (it, ctx)
            d_stage2(*prev)
            tf_ = dctx["tf"]
            store(s_cat.rearrange("(t p) n -> p t n", p=128)[:, :, 512:1024], catd[:], [tf_])
            barrier()

        if stage == 2:
            with nc.sbuf_tensor("dbgt2", [128, NT, D], BF16) as dt_:
                sm = kb.new_sem("dbgs2")
                tk = kb.dma("sync", dt_[:], s_cat.rearrange("(t p) n -> p t n", p=128), sm)
                store(dbg_out.rearrange("(t p) n -> p t n", p=128), dt_[:], [tk])
                finish()
            nc.used_inputs = list(used_inputs)
            return nc

        def outproj_ln(w_dram, src_dram_tm, src_dram_fm, xres_dram, li, out_dram, tag):
            with ExitStack() as pes:
                XB = pes.enter_context(nc.sbuf_tensor(f"XB{tag}", [128, 8, T], BF16))
                wo = pes.enter_context(nc.sbuf_tensor(f"wo{tag}", [128, 8, D], BF16))
                wsem = kb.new_sem(f"wos{tag}")
                wv = w_dram.rearrange("(c p) n -> p c n", p=128)
                wtok = None
                for c in range(8):
                    wtok = kb.dma("gpsimd", wo[:, c, :], wv[:, c, :], wsem)
                gB, bB, gbtok = load_ln_params(pes, li)
                R = LNRings(pes, tag)
                xready = []
                xtile = {}
                if src_dram_tm is not None:
                    cbufs = [pes.enter_context(nc.sbuf_tensor(f"cb{tag}{i}", [128, D], BF16)) for i in range(2)]
                    csems = [kb.new_sem(f"cbs{tag}{i}") for i in range(2)]
                    cr = Ring(cbufs)
                    for t in range(NT):
                        b_, cb, w = cr.get()
                        lt = kb.dma("sync", cb[:], src_dram_tm[t * 128:(t + 1) * 128, :], csems[b_], waits=w)
                        tks = to_fm(cb, t, XB, 8, [lt])
                        cr.rel(b_, tks[-1])
                        xtile[t] = tks
                else:
                    fsem = kb.new_sem(f"fms{tag}")
                    xready = [kb.dma("sync", XB[:], src_dram_fm.rearrange("(c p) t -> p c t", p=128), fsem)]
                PSO = PSBanks(banks[4:8])
                ybuf = pes.enter_context(nc.sbuf_tensor(f"ybuf{tag}", [128, NT, D], F32))
                rels = {}

                def get_halves(t):
                    halves = []
                    rels[t] = []
                    for dh in range(2):
                        b_, bank, w = PSO.get()
                        mt = None
                        for kc in range(8):
                            mt = kb.op("tensor", lambda e, kc=kc: e.matmul(
                                bank[:, :], lhsT=XB[:, kc, t * 128:(t + 1) * 128], rhs=wo[:, kc, dh * 512:(dh + 1) * 512],
                                start=(kc == 0), stop=(kc == 7)),
                                waits=list(w) + [wtok] + list(xready) + list(xtile.get(t, [])), sig=(kc == 7))
                        halves.append((bank[:, :], [mt]))
                        rels[t].append(b_)
                    return halves

                def rel_cb(t, toks):
                    for b_, tk in zip(rels[t], toks):
                        PSO.rel(b_, tk)

                resid_ln_all(get_halves, ybuf, xres_dram, gB, bB, gbtok, out_dram, XA, R, rel_cb)
                barrier()

        outproj_ln(I("w_out0"), s_cat, None, I("x_in"), 0, s_x1, "d0")

        if stage == 3:
            with nc.sbuf_tensor("dbgt3", [128, NT, D], F32) as dt_:
                sm = kb.new_sem("dbgs3")
                tk = kb.dma("sync", dt_[:], s_x1.rearrange("(t p) n -> p t n", p=128), sm)
                store(dbg_out.rearrange("(t p) n -> p t n", p=128), dt_[:], [tk])
                finish()
            nc.used_inputs = list(used_inputs)
            return nc

        def ffn_ln(experts, comb, xres_dram, li, out_dram, out_XT, tag, pre_compute=None):
            with ExitStack() as pes:
                sb = lambda nm, shp, dt: pes.enter_context(nc.sbuf_tensor(nm + tag, shp, dt))
                acc = sb("acc", [128, NT, D], F32)
                hTb = [sb(f"hT{i}", [128, 4, T], BF16) for i in range(2)]
                with ExitStack() as wes:
                    wsb = lambda nm, shp, dt: wes.enter_context(nc.sbuf_tensor(nm + tag, shp, dt))
                    wgb = [wsb(f"wg{i}", [128, 8, 512], BF16) for i in range(2)]
                    wub = [wsb(f"wu{i}", [128, 8, 512], BF16) for i in range(2)]
                    wdb = [wsb(f"wd{i}", [128, 4, D], BF16) for i in range(2)]
                    sgr = Ring([wsb(f"sg{i}", [128, 512], F32) for i in range(2)])
                    sems_gu = [kb.new_sem(f"ffgu{tag}{i}") for i in range(2)]
                    sems_d = [kb.new_sem(f"ffd{tag}{i}") for i in range(2)]
                    free_gu = [[], []]
                    free_d = [[], []]
                    items = [(ex, gi) for ex in experts for gi in range(len(FGROUPS))]
                    n_items = len(items)

                    def load_gu(idx):
                        (wg, wu, wd, e_), gi = items[idx]
                        f0, f1 = FGROUPS[gi]
                        nf = f1 - f0
                        s_ = idx % 2
                        w = free_gu[s_]
                        free_gu[s_] = []
                        kb.dma("gpsimd", wgb[s_][:, :, 0:nf * 128],
                               wg.rearrange("(c p) n -> p c n", p=128)[:, :, f0 * 128:f1 * 128], sems_gu[s_], waits=w)
                        return kb.dma("gpsimd", wub[s_][:, :, 0:nf * 128],
                                      wu.rearrange("(c p) n -> p c n", p=128)[:, :, f0 * 128:f1 * 128], sems_gu[s_])

                    def load_d(idx):
                        (wg, wu, wd, e_), gi = items[idx]
                        f0, f1 = FGROUPS[gi]
                        nf = f1 - f0
                        s_ = idx % 2
                        w = free_d[s_]
                        free_d[s_] = []
                        return kb.dma("gpsimd", wdb[s_][:, 0:nf, :],
                                      wd[f0 * 128:f1 * 128, :].rearrange("(c p) n -> p c n", p=128), sems_d[s_], waits=w)

                    PSG = PSBanks(banks[0:4])
                    PSO = PSBanks(banks[4:8])
                    acc_tok = [[None, None] for _ in range(NT)]
                    lg = {}
                    ld = {}

                    def emit_GU(idx):
                        (wg, wu, wd, e_), gi = items[idx]
                        f0, f1 = FGROUPS[gi]
                        nf = f1 - f0
                        s_ = idx % 2
                        hT = hTb[idx % 2]
                        hlast = None
                        mlast = None
                        for fc in range(nf):
                            for tt in range(4):
                                bg, bankg, wg_ = PSG.get()
                                bu, banku, wu_ = PSG.get()
                                mg = mu = None
                                for kc in range(8):
                                    mg = kb.op("tensor", lambda e, kc=kc: e.matmul(
                                        bankg[:, :], lhsT=wgb[s_][:, kc, fc * 128:(fc + 1) * 128],
                                        rhs=XA[:, kc, tt * 512:(tt + 1) * 512], start=(kc == 0), stop=(kc == 7)),
                                        waits=list(wg_) + [lg[idx]], sig=(kc == 7))
                                for kc in range(8):
                                    mu = kb.op("tensor", lambda e, kc=kc: e.matmul(
                                        banku[:, :], lhsT=wub[s_][:, kc, fc * 128:(fc + 1) * 128],
                                        rhs=XA[:, kc, tt * 512:(tt + 1) * 512], start=(kc == 0), stop=(kc == 7)),
                                        waits=list(wu_), sig=(kc == 7))
                                mlast = mu
                                sb_, sg, sw = sgr.get()
                                ta = kb.op("scalar", lambda e: e.activation(out=sg[:], in_=bankg[:, :], func=AF.Silu),
                                           waits=[mg] + list(sw), sig=True)
                                PSG.rel(bg, ta)
                                hlast = kb.op("vector", lambda e: e.tensor_tensor(
                                    out=hT[:, fc, tt * 512:(tt + 1) * 512], in0=sg[:], in1=banku[:, :], op=ALU.mult),
                                    waits=[ta, mu], sig=True)
                                PSG.rel(bu, hlast)
                                sgr.rel(sb_, hlast)
                        free_gu[s_].append(mlast)
                        return hlast

                    def emit_D(idx, hlast):
                        (wg, wu, wd, e_), gi = items[idx]
                        f0, f1 = FGROUPS[gi]
                        nf = f1 - f0
                        s_ = idx % 2
                        hT = hTb[idx % 2]
                        dlast = None
                        for t in range(NT):
                            for dh in range(2):
                                bo, banko, wo_ = PSO.get()
                                for fc in range(nf):
                                    dlast = kb.op("tensor", lambda e, fc=fc: e.matmul(
                                        banko[:, :], lhsT=hT[:, fc, t * 128:(t + 1) * 128],
                                        rhs=wdb[s_][:, fc, dh * 512:(dh + 1) * 512], start=(fc == 0), stop=(fc == nf - 1)),
                                        waits=list(wo_) + [hlast, ld[idx]], sig=(fc == nf - 1))
                                a_ap = acc[:, t, dh * 512:(dh + 1) * 512]
                                prev = acc_tok[t][dh]
                                if prev is None:
                                    if comb is None:
                                        tk = kb.op("vector", lambda e: e.tensor_copy(out=a_ap, in_=banko[:, :]),
                                                   waits=[dlast], sig=True)
                                    else:
                                        tk = kb.op("vector", lambda e: e.tensor_scalar(
                                            out=a_ap, in0=banko[:, :], scalar1=comb[:, t, e_:e_ + 1], scalar2=None,
                                            op0=ALU.mult), waits=[dlast, pre_tok], sig=True)
                                else:
                                    if comb is None:
                                        tk = kb.op("vector", lambda e: e.tensor_tensor(out=a_ap, in0=banko[:, :], in1=a_ap,
                                                                                       op=ALU.add),
                                                   waits=[dlast, prev], sig=True)
                                    else:
                                        tk = kb.op("vector", lambda e: e.scalar_tensor_tensor(
                                            out=a_ap, in0=banko[:, :], scalar=comb[:, t, e_:e_ + 1], in1=a_ap,
                                            op0=ALU.mult, op1=ALU.add), waits=[dlast, prev], sig=True)
                                acc_tok[t][dh] = tk
                                PSO.rel(bo, tk)
                        free_d[s_].append(dlast)

                    lg[0] = load_gu(0)
                    ld[0] = load_d(0)
                    if n_items > 1:
                        lg[1] = load_gu(1)
                        ld[1] = load_d(1)
                    pre_tok = pre_compute(acc) if pre_compute is not None else None
                    hl = {0: emit_GU(0)}
                    for idx in range(n_items):
                        if idx + 1 < n_items:
                            hl[idx + 1] = emit_GU(idx + 1)
                        if idx + 2 < n_items:
                            lg[idx + 2] = load_gu(idx + 2)
                        emit_D(idx, hl[idx])
                        if idx + 2 < n_items:
                            ld[idx + 2] = load_d(idx + 2)
                    barrier()
                gB, bB, gbtok = load_ln_params(pes, li)
                R = LNRings(pes, tag)
                resid_ln_all(lambda t: [(acc[:, t, dh * 512:(dh + 1) * 512], [acc_tok[t][dh]]) for dh in range(2)],
                             acc, xres_dram, gB, bB, gbtok, out_dram, out_XT, R)
                barrier()

        ffn_ln([(I("ffn_wg"), I("ffn_wu"), I("ffn_wd"), None)], None, s_x1, 1, s_x2, XA, "f0")

        if stage == 4:
            with nc.sbuf_tensor("dbgt4", [128, NT, D], F32) as dt_:
                sm = kb.new_sem("dbgs4")
                tk = kb.dma("sync", dt_[:], s_x2.rearrange("(t p) n -> p t n", p=128), sm)
                store(dbg_out.rearrange("(t p) n -> p t n", p=128), dt_[:], [tk])
                finish()
            nc.used_inputs = list(used_inputs)
            return nc

        with ExitStack() as pes:
            sb = lambda nm, shp, dt: pes.enter_context(nc.sbuf_tensor(nm, shp, dt))
            w1 = sb("w1", [128, 8, ODD_IN], BF16)
            w1r = sb("w1r", [128, 8, 32], BF16)
            gq = sb("gq", [128, 384], F32)
            cT = sb("cT", [128, 3, T], BF16)
            cosF = sb("cosF", [96, T], F32)
            sinF = sb("sinF", [96, T], F32)
            KR = sb("KR", [96, T], BF16)
            wuq = sb("wuq", [128, 2, 1536], BF16)
            wuqr = sb("wuqr", [128, 2, 16, 96], BF16)
            wukv = sb("wukv", [128, 16, 128], BF16)
            vaug = sb("vaugm", [128, NT, 16, 66], BF16)
            ssq = sb("ssq", [128, NT, 4], F32)
            rsq = sb("rsq", [128, NT, 4], F32)
            gsem = kb.new_sem("mla_g")
            ssem = kb.new_sem("mla_s")
            for c in range(8):
                kb.dma("gpsimd", w1[:, c, :], I("w_in1").rearrange("(c p) n -> p c n", p=128)[:, c, :], gsem)
            kb.dma("gpsimd", w1r[:], I("w_in1_rot").rearrange("(c p) n -> p c n", p=128), gsem)
            kb.dma("gpsimd", wuq[:], I("w_uq").rearrange("(c p) n -> p c n", p=128), gsem)
            tz = kb.op("vector", lambda e: e.memset(wuqr[:].rearrange("p a h r -> p (a h r)"), 0.0), sig=True)
            for c in range(2):
                kb.dma("gpsimd", wuqr[:, c, :, 64:96],
                       I("w_uq_rot").rearrange("(c p) (h r) -> p c h r", p=128, r=32)[:, c, :, :], gsem,
                       waits=[tz])
            gtok = kb.dma("gpsimd", wukv[:].rearrange("p h d -> p (h d)"), I("w_ukv")[:, :], gsem)
            kb.dma("sync", gq[:, 0:256], I("qn_gain")[0, :].partition_broadcast(128), ssem)
            kb.dma("sync", gq[:, 256:384], I("kvn_gain")[0, :].partition_broadcast(128), ssem)
            kb.dma("sync", cosF[:], I("c_cosF")[:, :], ssem)
            stok_ = kb.dma("sync", sinF[:], I("c_sinF")[:, :], ssem)
            t_ones = kb.op("gpsimd", lambda e: e.memset(vaug[:, :, :, 64:65], 1.0), sig=True)
            LW = [gtok, stok_]
            PS_P = PSBanks(banks[6:8])
            csb_all = sb("csb_all", [128, NT, 384], F32)
            cnr = Ring([sb(f"cnb{i}", [128, 384], BF16) for i in range(2)])
            jk = sb("jkm", [128, 2, 256], F32)
            jk_tok = [None, None]
            for t in range(NT):
                b_, bank, w = PS_P.get()
                mt = None
                for kc in range(8):
                    mt = kb.op("tensor", lambda e, kc=kc: e.matmul(
                        bank[:, 0:384], lhsT=XA[:, kc, t * 128:(t + 1) * 128], rhs=w1[:, kc, 0:384],
                        start=(kc == 0), stop=(kc == 7)), waits=list(w) + LW, sig=(kc == 7))
                tcp = kb.op("scalar", lambda e: e.copy(out=csb_all[:, t, :], in_=bank[:, 0:384]), waits=[mt], sig=True)
                PS_P.rel(b_, tcp)
                ta = kb.op("vector", lambda e: e.scalar_tensor_tensor(
                    out=jk[:, 0, :], in0=csb_all[:, t, 0:256], scalar=1.0, in1=csb_all[:, t, 0:256], op0=ALU.mult,
                    op1=ALU.mult, accum_out=ssq[:, t, 0:1]), waits=[tcp, jk_tok[0]], sig=True)
                jk_tok[0] = ta
                tb = kb.op("vector", lambda e: e.scalar_tensor_tensor(
                    out=jk[:, 1, 0:128], in0=csb_all[:, t, 256:384], scalar=1.0, in1=csb_all[:, t, 256:384],
                    op0=ALU.mult, op1=ALU.mult, accum_out=ssq[:, t, 1:2]), waits=[tcp, jk_tok[1]], sig=True)
                jk_tok[1] = tb
            tl1 = kb.op("scalar", lambda e: e.activation(out=rsq[:, :, 0], in_=ssq[:, :, 0], func=AF.Ln,
                                                         bias=eps6[:, 0:1], scale=1.0 / 256), waits=[ta, tb] + CW, sig=True)
            tl2 = kb.op("scalar", lambda e: e.activation(out=rsq[:, :, 1], in_=ssq[:, :, 1], func=AF.Ln,
                                                         bias=eps6[:, 0:1], scale=1.0 / 128), waits=[tl1], sig=True)
            te_ = kb.op("scalar", lambda e: e.activation(out=rsq[:, :, 2:4], in_=rsq[:, :, 0:2], func=AF.Exp,
                                                         scale=-0.5), waits=[tl2], sig=True)
            for t in range(NT):
                nb_, cn, nw = cnr.get()
                kb.op("vector", lambda e: e.scalar_tensor_tensor(
                    out=cn[:, 0:256], in0=csb_all[:, t, 0:256], scalar=rsq[:, t, 2:3], in1=gq[:, 0:256], op0=ALU.mult,
                    op1=ALU.mult), waits=[te_] + list(nw) + LW)
                tn = kb.op("vector", lambda e: e.scalar_tensor_tensor(
                    out=cn[:, 256:384], in0=csb_all[:, t, 256:384], scalar=rsq[:, t, 3:4], in1=gq[:, 256:384],
                    op0=ALU.mult, op1=ALU.mult), sig=True)
                tks_prev = tks if t > 0 else []
                tks = to_fm(cn, t, cT, 3, [tn])
                cnr.rel(nb_, tks[-1])
            cT_ready = list(tks_prev) + list(tks)
            t1r = Ring([sb(f"t1m{i}", [96, 512], F32) for i in range(2)])
            t2r = Ring([sb(f"t2m{i}", [96, 512], F32) for i in range(2)])
            kr_tok = None
            for tt in range(4):
                ba, bankA, wa = PS_P.get()
                bb, bankB, wb = PS_P.get()
                ma = mb_ = None
                for kc in range(8):
                    ma = kb.op("tensor", lambda e, kc=kc: e.matmul(
                        bankA[64:96, :], lhsT=w1[:, kc, 384:416], rhs=XA[:, kc, tt * 512:(tt + 1) * 512],
                        start=(kc == 0), stop=(kc == 7)), waits=list(wa) + LW, sig=(kc == 7))
                for kc in range(8):
                    mb_ = kb.op("tensor", lambda e, kc=kc: e.matmul(
                        bankB[64:96, :], lhsT=w1r[:, kc, 0:32], rhs=XA[:, kc, tt * 512:(tt + 1) * 512],
                        start=(kc == 0), stop=(kc == 7)), waits=list(wb), sig=(kc == 7))
                i1, t1, w1_ = t1r.get()
                i2, t2, w2_ = t2r.get()
                ka = kb.op("vector", lambda e: e.tensor_tensor(out=t1[64:96, :], in0=bankA[64:96, :],
                                                               in1=cosF[64:96, tt * 512:(tt + 1) * 512], op=ALU.mult),
                           waits=[ma] + list(w1_) + LW, sig=True)
                PS_P.rel(ba, ka)
                kbk = kb.op("vector", lambda e: e.tensor_tensor(out=t2[64:96, :], in0=bankB[64:96, :],
                                                                in1=sinF[64:96, tt * 512:(tt + 1) * 512], op=ALU.mult),
                            waits=[mb_] + list(w2_), sig=True)
                PS_P.rel(bb, kbk)
                kr_tok = kb.op("gpsimd", lambda e: e.tensor_tensor(out=KR[64:96, tt * 512:(tt + 1) * 512],
                                                                   in0=t1[64:96, :], in1=t2[64:96, :], op=ALU.add),
                               waits=[ka, kbk], sig=True)
                t1r.rel(i1, kr_tok)
                t2r.rel(i2, kr_tok)
            v_tok = None
            for t in range(NT):
                for hf in range(2):
                    b_, bank, w = PS_P.get()
                    mt = kb.op("tensor", lambda e: e.matmul(
                        bank[:, :], lhsT=cT[:, 2, t * 128:(t + 1) * 128], rhs=wukv[:, hf * 8:(hf + 1) * 8, 64:128],
                        start=True, stop=True), waits=list(w) + LW + list(cT_ready), sig=True)
                    v_tok = evac(vaug[:, t, hf * 8:(hf + 1) * 8, 0:64],
                                 bank[:, :].rearrange("p (h d) -> p h d", d=64), [mt, t_ones])
                    PS_P.rel(b_, v_tok)
            PS_O = PSBanks(banks[4:6])
            QTr = Ring([sb(f"QT{i}", [96, T], BF16) for i in range(2)])
            KTr = Ring([sb(f"KT{i}", [96, T], BF16) for i in range(2)])
            ptr = Ring([sb(f"ptm{i}", [128, 512], BF16) for i in range(3)])
            recr = Ring([sb(f"rec{i}", [65, 512], F32) for i in range(2)])
            bcr = Ring([sb(f"bcs{i}", [64, 512], F32) for i in range(2)])
            onr = Ring([sb(f"on{i}", [64, 512], BF16) for i in range(2)])
            on_sems = [new_store_sem(f"ons{i}") for i in range(2)]
            SC = 96 ** -0.5
            head_ctx = {}

            def mla_prep(h):
                qi, QT, qw = QTr.get()
                ki, KT, kw = KTr.get()
                qk_toks = []
                for tt in range(4):
                    ba, bankA, wa = PS_P.get()
                    bb, bankB, wb = PS_P.get()
                    ma = mb_ = None
                    for kc in range(2):
                        ma = kb.op("tensor", lambda e, kc=kc: e.matmul(
                            bankA[0:96, :], lhsT=wuq[:, kc, h * 96:(h + 1) * 96], rhs=cT[:, kc, tt * 512:(tt + 1) * 512],
                            start=(kc == 0), stop=(kc == 1)), waits=list(wa) + LW + list(cT_ready), sig=(kc == 1))
                    for kc in range(2):
                        mb_ = kb.op("tensor", lambda e, kc=kc: e.matmul(
                            bankB[0:96, :], lhsT=wuqr[:, kc, h, :], rhs=cT[:, kc, tt * 512:(tt + 1) * 512],
                            start=(kc == 0), stop=(kc == 1)), waits=list(wb), sig=(kc == 1))
                    i1, t1, w1_ = t1r.get()
                    i2, t2, w2_ = t2r.get()
                    ka = kb.op("vector", lambda e: e.tensor_tensor(out=t1[:, :], in0=bankA[0:96, :],
                                                                   in1=cosF[:, tt * 512:(tt + 1) * 512], op=ALU.mult),
                               waits=[ma] + list(w1_), sig=True)
                    PS_P.rel(ba, ka)
                    kbk = kb.op("vector", lambda e: e.tensor_tensor(out=t2[:, :], in0=bankB[0:96, :],
                                                                    in1=sinF[:, tt * 512:(tt + 1) * 512], op=ALU.mult),
                                waits=[mb_] + list(w2_), sig=True)
                    PS_P.rel(bb, kbk)
                    tq = kb.op("gpsimd", lambda e: e.tensor_tensor(out=QT[:, tt * 512:(tt + 1) * 512], in0=t1[:, :],
                                                                   in1=t2[:, :], op=ALU.add),
                               waits=[ka, kbk] + list(qw), sig=True)
                    t1r.rel(i1, tq)
                    t2r.rel(i2, tq)
                    bk, bankK, wk = PS_P.get()
                    mk = kb.op("tensor", lambda e: e.matmul(
                        bankK[0:64, :], lhsT=wukv[:, h, 0:64], rhs=cT[:, 2, tt * 512:(tt + 1) * 512],
                        start=True, stop=True), waits=list(wk) + LW + list(cT_ready), sig=True)
                    tk_ = kb.op("vector", lambda e: e.tensor_copy(out=KT[0:64, tt * 512:(tt + 1) * 512],
                                                                  in_=bankK[0:64, :]), waits=[mk] + list(kw), sig=True)
                    PS_P.rel(bk, tk_)
                    qk_toks += [tq, tk_]
                tkr = kb.op("gpsimd", lambda e: e.tensor_copy(out=KT[64:96, :], in_=KR[64:96, :]),
                            waits=[kr_tok] + list(kw), sig=True)
                qk_toks.append(tkr)
                head_ctx[h] = (qi, QT, ki, KT, qk_toks)

            deferred = []
            po_ctx = {}

            def flush_deferred():
                while deferred:
                    deferred.pop(0)()

            PS_S2 = PSBanks([pds[0], pds[1]])
            pt2r = Ring([sb(f"pt2m{i}", [128, 1024], BF16) for i in range(3)])

            def mla_stage1(it):
                h, qt, kp = it
                if qt == 0 and kp == 0 and h not in head_ctx:
                    mla_prep(h)
                if qt == 2 and kp == 0 and h + 1 < 16:
                    mla_prep(h + 1)
                if kp == 5:
                    flush_deferred()
                qi, QT, ki, KT, qk_toks = head_ctx[h]
                b_, pd, w = PS_S2.get()
                mt = None
                for j in range(2):
                    kc = 2 * kp + j
                    mt = kb.op("tensor", lambda e: e.matmul(
                        pd[:, j * 512:(j + 1) * 512], lhsT=KT[0:96, kc * 128:(kc + 1) * 128],
                        rhs=QT[0:96, qt * 512:(qt + 1) * 512], start=True, stop=True),
                        waits=list(w) + qk_toks, sig=(j == 1))
                pb_, PT, pw = pt2r.get()
                t2_ = kb.op("scalar", lambda e: e.activation(out=PT[:], in_=pd[:, :], func=AF.Exp, scale=SC),
                            waits=[mt] + list(pw), sig=True)
                PS_S2.rel(b_, t2_)
                return (pb_, PT, t2_)

            def mla_stage2(it, ctx):
                h, qt, kp = it
                pb_, PT, t2_ = ctx
                if kp == 0:
                    po_ctx[(h, qt)] = PS_O.get()
                bo, po, wo_ = po_ctx[(h, qt)]
                pv = None
                for j in range(2):
                    kc = 2 * kp + j
                    pv = kb.op("tensor", lambda e: e.matmul(
                        po[0:65, :], lhsT=vaug[:, kc, h, 0:65], rhs=PT[:, j * 512:(j + 1) * 512], start=(kc == 0),
                        stop=(kc == NT - 1)), waits=[t2_, v_tok] + (list(wo_) if kc == 0 else []), sig=(j == 1))
                pt2r.rel(pb_, pv)
                if kp == 7:
                    ri, rec, rw = recr.get()
                    trc = kb.op("vector", lambda e: e.reciprocal(out=rec[64:65, :], in_=po[64:65, :]),
                                waits=[pv] + list(rw), sig=True)
                    qi, QT, ki, KT, qk_toks = head_ctx[h]
                    if qt == 3:
                        QTr.rel(qi, pv)
                        KTr.rel(ki, pv)

                    def fin():
                        bc_i, bcb, bcw = PS_P.get()
                        mbc = kb.op("tensor", lambda e: e.matmul(bcb[0:64, :], lhsT=ones_f[64:65, 0:64],
                                                                 rhs=rec[64:65, :], start=True, stop=True),
                                    waits=[trc] + list(bcw) + CW, sig=True)
                        recr.rel(ri, mbc)
                        si, bcs, sw = bcr.get()
                        tbs = kb.op("vector", lambda e: e.tensor_copy(out=bcs[:], in_=bcb[0:64, :]),
                                    waits=[mbc] + list(sw), sig=True)
                        PS_P.rel(bc_i, tbs)
                        oi, on, ow = onr.get()
                        ton = kb.op("vector", lambda e: e.tensor_tensor(out=on[:], in0=po[0:64, :], in1=bcs[:],
                                                                        op=ALU.mult), waits=[tbs] + list(ow), sig=True)
                        PS_O.rel(bo, ton)
                        bcr.rel(si, ton)
                        stk = store(s_attnT[h * 64:(h + 1) * 64, qt * 512:(qt + 1) * 512], on[:], [ton], on_sems[oi])
                        onr.rel(oi, stk)
                    deferred.append(fin)

            items = [(h, qt, kp) for h in range(16) for qt in range(4) for kp in range(NT // 2)]
            prev = None
            for it in items:
                ctx = mla_stage1(it)
                if prev is not None:
                    mla_stage2(*prev)
                prev = (it, ctx)
            mla_stage2(*prev)
            flush_deferred()
            barrier()

        if stage == 5:
            with nc.sbuf_tensor("dbgt5", [128, 8, T], BF16) as dt_:
                sm = kb.new_sem("dbgs5")
                tk = kb.dma("sync", dt_[:], s_attnT.rearrange("(c p) t -> p c t", p=128), sm)
                store(dbg_out.rearrange("(c p) t -> p c t", p=128), dt_[:], [tk])
                finish()
            nc.used_inputs = list(used_inputs)
            return nc

        outproj_ln(I("w_out1"), None, s_attnT, s_x2, 2, s_x3, "d1")

        if stage == 6:
            with nc.sbuf_tensor("dbgt6", [128, NT, D], F32) as dt_:
                sm = kb.new_sem("dbgs6")
                tk = kb.dma("sync", dt_[:], s_x3.rearrange("(t p) n -> p t n", p=128), sm)
                store(dbg_out.rearrange("(t p) n -> p t n", p=128), dt_[:], [tk])
                finish()
            nc.used_inputs = list(used_inputs)
            return nc

        comb = kb.sbuf("comb", [128, NT, NE], F32)
        logits = kb.sbuf("logits", [128, NT, NE], F32)
        mx = kb.sbuf("mx", [128, NT, 8], F32)
        wts = kb.sbuf("wts", [128, 6, NT], F32)
        eq = kb.sbuf("eq", [128, NT, 2, NE], F32)

        def router_compute(scr):
            wrB = scr[:, 0:8, :]
            x3b = [scr[:, 8, :], scr[:, 9, :]]
            x3free = [[], []]
            jr = scr[:, 10:12, :]
            x3s = [kb.new_sem(f"x3s{i}") for i in range(2)]
            jr_tok = [None, None]
            rs = kb.new_sem("rts")
            rtok = kb.dma("sync", wrB.rearrange("p e d -> p (e d)"), I("router_wT")[0, :].partition_broadcast(128), rs)
            lg_tok = None
            n_ = 0
            for t in range(NT):
                xi = t % 2
                x3 = x3b[xi]
                lt = kb.dma("sync", x3, s_x3[t * 128:(t + 1) * 128, :], x3s[xi], waits=x3free[xi])
                x3free[xi] = []
                for e_ in range(NE):
                    lg_tok = kb.op("vector", lambda e, e_=e_: e.scalar_tensor_tensor(
                        out=jr[:, n_ % 2, :], in0=x3, scalar=1.0, in1=wrB[:, e_, :], op0=ALU.mult, op1=ALU.mult,
                        accum_out=logits[:, t, e_:e_ + 1]), waits=[lt, rtok, jr_tok[n_ % 2]], sig=True)
                    jr_tok[n_ % 2] = lg_tok
                    n_ += 1
                x3free[xi].append(lg_tok)
            tm = None
            for t in range(NT):
                tm = kb.op("vector", lambda e: e.max(out=mx[:, t, :], in_=logits[:, t, :]), waits=[lg_tok], sig=True)
            td_ = kb.op("vector", lambda e: e.tensor_tensor(out=wts[:, 0, :], in0=mx[:, :, 1], in1=mx[:, :, 0],
                                                            op=ALU.subtract), waits=[tm], sig=True)
            te2 = kb.op("scalar", lambda e: e.activation(out=wts[:, 1, :], in_=wts[:, 0, :], func=AF.Exp), waits=[td_],
                        sig=True)
            tdn = kb.op("vector", lambda e: e.tensor_scalar(out=wts[:, 2, :], in0=wts[:, 1, :], scalar1=1.0, scalar2=None,
                                                            op0=ALU.add), waits=[te2], sig=True)
            tw1 = kb.op("vector", lambda e: e.reciprocal(out=wts[:, 3, :], in_=wts[:, 2, :]), waits=[tdn], sig=True)
            tw2 = kb.op("vector", lambda e: e.tensor_tensor(out=wts[:, 4, :], in0=wts[:, 1, :], in1=wts[:, 3, :],
                                                            op=ALU.mult), waits=[tw1], sig=True)
            comb_tok = None
            for t in range(NT):
                ea = kb.op("vector", lambda e: e.tensor_scalar(out=eq[:, t, 0, :], in0=logits[:, t, :],
                                                               scalar1=mx[:, t, 0:1], scalar2=wts[:, 3, t:t + 1],
                                                               op0=ALU.is_equal, op1=ALU.mult), waits=[tw2], sig=True)
                eb = kb.op("vector", lambda e: e.tensor_scalar(out=eq[:, t, 1, :], in0=logits[:, t, :],
                                                               scalar1=mx[:, t, 1:2], scalar2=wts[:, 4, t:t + 1],
                                                               op0=ALU.is_equal, op1=ALU.mult), sig=True)
                comb_tok = kb.op("vector", lambda e: e.tensor_tensor(out=comb[:, t, :], in0=eq[:, t, 0, :],
                                                                     in1=eq[:, t, 1, :], op=ALU.add),
                                 waits=[ea, eb], sig=True)
            return comb_tok

        experts = [(I("moe_wg")[e_], I("moe_wu")[e_], I("moe_wd")[e_], e_) for e_ in range(NE)]
        ffn_ln(experts, comb, s_x3, 3, y_out, None, "f1", pre_compute=router_compute)
        if stage == 7:
            with nc.sbuf_tensor("dbgt7", [128, NT, D], F32) as dt_:
                sm = kb.new_sem("dbgs7")
                tk = kb.dma("sync", dt_[:], y_out.rearrange("(t p) n -> p t n", p=128), sm)
                store(dbg_out.rearrange("(t p) n -> p t n", p=128), dt_[:], [tk])
        finish()
    nc.used_inputs = list(used_inputs)
    return nc


def host_inputs(inp):
    f = lambda a: np.ascontiguousarray(np.asarray(a, dtype=np.float32))
    c = host_consts()
    sh = {}
    sh["even_w_in"] = f(inp["even_w_in"][0])
    gu = np.asarray(inp["gla_gate_up"][0], np.float32)
    gbd = np.zeros((32, 512), np.float32)
    gbd[0:16, 0:256] = gu[0]
    gbd[16:32, 256:512] = gu[1]
    sh["gate_bd"] = gbd
    sh["gate_bias"] = f(np.asarray(inp["gla_gate_bias"][0]).reshape(1, 512))
    sh["gla_gain"] = f(np.asarray(inp["gla_norm_gain"][0]).reshape(1, 128))
    sh["diff_lambda"] = f(np.asarray(inp["diff_lambda"][0]).reshape(1, 256))
    sh["diff_gain"] = f(np.asarray(inp["diff_norm_gain"][0]).reshape(1, 128))
    sh["even_w_out"] = f(inp["even_w_out"][0])
    sh["ffn_w_gate"] = f(inp["ffn_w_gate"][0])
    sh["ffn_w_up"] = f(inp["ffn_w_up"][0])
    sh["ffn_w_down"] = f(inp["ffn_w_down"][0])
    w1 = np.asarray(inp["odd_w_in"][0], np.float32)
    sh["odd_w_in"] = f(w1)
    sh["odd_w_in_rot"] = f(np.concatenate([w1[:, 400:416], w1[:, 384:400]], axis=1))
    sh["mla_q_gain"] = f(np.asarray(inp["mla_q_norm_gain"][0]).reshape(1, 256))
    sh["mla_kv_gain"] = f(np.asarray(inp["mla_kv_norm_gain"][0]).reshape(1, 128))
    wq = np.asarray(inp["mla_w_uq"][0], np.float32)
    sh["mla_w_uq"] = f(wq)
    wq3 = wq.reshape(256, 16, 96)
    sh["mla_w_uq_rot"] = f(np.concatenate([wq3[:, :, 80:96], wq3[:, :, 64:80]], axis=2).reshape(256, 512))
    sh["mla_w_ukv"] = f(inp["mla_w_ukv"][0])
    sh["odd_w_out"] = f(inp["odd_w_out"][0])
    sh["router_wT"] = f(np.asarray(inp["router_w"][0]).T.reshape(1, NE * D))
    sh["moe_w_gate"] = f(inp["moe_w_gate"][0])
    sh["moe_w_up"] = f(inp["moe_w_up"][0])
    sh["moe_w_down"] = f(inp["moe_w_down"][0])
    sh["ln_gain"] = f(np.asarray(inp["ln_gain"]).reshape(4, D))
    sh["ln_bias"] = f(np.asarray(inp["ln_bias"]).reshape(4, D))
    table = np.asarray(inp["rel_bias_table"], np.float32)
    kl = np.arange(128)[:, None, None]
    m = np.arange(6)[None, :, None]
    ql = np.arange(512)[None, None, :]
    rel = 128 * (m - 1) + kl - ql
    bidx = _bucket(rel)
    toe = table[bidx]
    sh["toe"] = f(np.transpose(toe, (0, 3, 1, 2)).reshape(128, 4 * 6 * 512))
    sh["far"] = f(np.stack([table[15, :], table[31, :]], axis=1).reshape(1, 8))
    for k in ["RF", "RB", "SU", "SL", "ident", "cosF", "sinF"]:
        sh["c_" + k] = c[k]
    return sh


_NC_CACHE = {}


def kernel(**inputs):
    sh = host_inputs(inputs)
    x = np.asarray(inputs["x"], np.float32)
    B = x.shape[0]
    if "nc" not in _NC_CACHE:
        _NC_CACHE["nc"] = build()
    nc = _NC_CACHE["nc"]
    in_maps = []
    for b in range(B):
        m = dict(sh)
        m["x"] = np.ascontiguousarray(x[b])
        in_maps.append({k: v for k, v in m.items() if k in nc.used_inputs})
    res = run_bass_kernel_spmd(nc, in_maps, core_ids=list(range(B)))
    return np.stack([np.asarray(r["y"], np.float32) for r in res.results], axis=0)
```

```python
import math
from contextlib import ExitStack

import numpy as np
import concourse.bass as bass
import concourse.mybir as mybir
from concourse.bass_utils import run_bass_kernel_spmd

F32 = mybir.dt.float32
BF16 = mybir.dt.bfloat16
AF = mybir.ActivationFunctionType
ALU = mybir.AluOpType

T = 2048
NT = 16
D = 1024
DFF = 2816
NE = 8
ALPHA = 4 ** 0.25
LAM_INIT = 0.8 - 0.6 * math.exp(-0.3 * 0)
EVEN_IN = 3104
ODD_IN = 416
FGROUPS = [(0, 4), (4, 8), (8, 12), (12, 16), (16, 19), (19, 22)]


class Sem:
    _n = 0

    def __init__(self, h):
        self.h = h
        self.v = 0
        Sem._n += 1
        self.uid = Sem._n


class KB:
    ENGS = ["sync", "scalar", "vector", "gpsimd", "tensor"]

    def __init__(self, nc, es):
        self.nc = nc
        self.es = es
        self.q = {e: [] for e in self.ENGS}
        self.waited = {e: {} for e in self.ENGS}
        self.esem = {e: self.new_sem("pg_" + e) for e in ["scalar", "vector", "gpsimd", "tensor"]}
        self.nsem = 0

    def new_sem(self, name):
        return Sem(self.es.enter_context(self.nc.semaphore(name)))

    def sbuf(self, name, shape, dt):
        return self.es.enter_context(self.nc.sbuf_tensor(name, shape, dt))

    def wait(self, eng, tok):
        if tok is None:
            return
        sem, val = tok
        key = sem.uid
        if self.waited[eng].get(key, 0) >= val:
            return
        self.waited[eng][key] = val
        getattr(self.nc, eng).wait_ge(sem.h, val)

    def op(self, eng, fn, waits=(), sig=False):
        for w in waits:
            self.wait(eng, w)
        ins = fn(getattr(self.nc, eng))
        if sig:
            s = self.esem[eng]
            s.v += 1
            ins.then_inc(s.h, 1)
            return (s, s.v)
        return None

    def dma(self, eng, out, in_, sem, waits=()):
        for w in waits:
            self.wait(eng, w)
        sem.v += 16
        getattr(self.nc, eng).dma_start(out=out, in_=in_).then_inc(sem.h, 16)
        return (sem, sem.v)


class PSBanks:
    def __init__(self, banks):
        self.banks = banks
        self.free = [[] for _ in banks]
        self.i = 0

    def get(self):
        b = self.i % len(self.banks)
        self.i += 1
        w = self.free[b]
        self.free[b] = []
        return b, self.banks[b], w

    def rel(self, b, tok):
        if tok is not None:
            self.free[b].append(tok)


class Ring:
    def __init__(self, bufs):
        self.bufs = bufs
        self.free = [[] for _ in bufs]
        self.i = 0

    def get(self):
        b = self.i % len(self.bufs)
        self.i += 1
        w = self.free[b]
        self.free[b] = []
        return b, self.bufs[b], w

    def rel(self, b, tok):
        if tok is not None:
            self.free[b].append(tok)


def _bucket(rel):
    half = 16
    max_exact = 8
    bucket = np.where(rel > 0, half, 0).astype(np.int32)
    n = np.abs(rel)
    n_large = max_exact + (np.log(np.maximum(n, max_exact).astype(np.float32) / max_exact)
                           / math.log(128 / max_exact) * (half - max_exact)).astype(np.int32)
    n_large = np.minimum(n_large, half - 1)
    return bucket + np.where(n < max_exact, n, n_large)


def host_consts():
    j = np.arange(128)[:, None]
    i = np.arange(128)[None, :]
    same = (j // 64) == (i // 64)
    c = {}
    triF = (same & (j <= i)).astype(np.float32)
    triB = (same & (j >= i)).astype(np.float32)
    cind = ((np.arange(128)[:, None] // 64) == np.arange(2)[None, :]).astype(np.float32)
    c["RF"] = np.concatenate([triF, cind], axis=1)
    c["RB"] = np.concatenate([triB, cind], axis=1)
    c["SU"] = (same & (j > i)).astype(np.float32)
    c["SL"] = (same & (j < i)).astype(np.float32)
    c["ident"] = np.eye(128, dtype=np.float32)
    half = 16
    inv = (10000.0 ** (-np.arange(half, dtype=np.float32) / half)).astype(np.float32)
    ang = np.arange(T, dtype=np.float32)[:, None] * inv[None, :]
    cos = np.cos(ang).astype(np.float32).T
    sin = np.sin(ang).astype(np.float32).T
    cosF = np.ones((96, T), np.float32)
    sinF = np.zeros((96, T), np.float32)
    cosF[64:80] = cos
    cosF[80:96] = cos
    sinF[64:80] = -sin
    sinF[80:96] = sin
    c["cosF"] = cosF
    c["sinF"] = sinF
    return c


def build(stage=99, dbg=None):
    import os
    nc = bass.Bass("TRN2", target_bir_lowering=False)
    es = ExitStack()
    with es:
        kb = KB(nc, es)

        def din(name, shape, dt=F32):
            return nc.dram_tensor(name, list(shape), dt, kind="ExternalInput").ap()

        def dscr(name, shape, dt):
            return nc.dram_tensor(name, list(shape), dt, kind="Internal").ap()

        IN_SHAPES = {
            "x_in": ("x", [T, D]),
            "w_in0": ("even_w_in", [D, EVEN_IN]),
            "gate_bd": ("gate_bd", [32, 512]),
            "gate_bias": ("gate_bias", [1, 512]),
            "gla_gain": ("gla_gain", [1, 128]),
            "diff_lam": ("diff_lambda", [1, 256]),
            "diff_gain": ("diff_gain", [1, 128]),
            "w_out0": ("even_w_out", [D, D]),
            "ffn_wg": ("ffn_w_gate", [D, DFF]),
            "ffn_wu": ("ffn_w_up", [D, DFF]),
            "ffn_wd": ("ffn_w_down", [DFF, D]),
            "w_in1": ("odd_w_in", [D, ODD_IN]),
            "w_in1_rot": ("odd_w_in_rot", [D, 32]),
            "qn_gain": ("mla_q_gain", [1, 256]),
            "kvn_gain": ("mla_kv_gain", [1, 128]),
            "w_uq": ("mla_w_uq", [256, 1536]),
            "w_uq_rot": ("mla_w_uq_rot", [256, 16 * 32]),
            "w_ukv": ("mla_w_ukv", [128, 2048]),
            "w_out1": ("odd_w_out", [D, D]),
            "router_wT": ("router_wT", [1, NE * D]),
            "moe_wg": ("moe_w_gate", [NE, D, DFF]),
            "moe_wu": ("moe_w_up", [NE, D, DFF]),
            "moe_wd": ("moe_w_down", [NE, DFF, D]),
            "ln_g": ("ln_gain", [4, D]),
            "ln_b": ("ln_bias", [4, D]),
            "toe_in": ("toe", [128, 4 * 6 * 512]),
            "far_in": ("far", [1, 8]),
            "c_RF": ("c_RF", [128, 130]),
            "c_RB": ("c_RB", [128, 130]),
            "c_SU": ("c_SU", [128, 128]),
            "c_SL": ("c_SL", [128, 128]),
            "c_ident": ("c_ident", [128, 128]),
            "c_cosF": ("c_cosF", [96, T]),
            "c_sinF": ("c_sinF", [96, T]),
        }
        in_cache = {}
        used_inputs = []

        def I(var):
            if var not in in_cache:
                name, shp = IN_SHAPES[var]
                in_cache[var] = nc.dram_tensor(name, list(shp), F32, kind="ExternalInput").ap()
                used_inputs.append(name)
            return in_cache[var]

        nc_used_inputs = used_inputs
        y_out = nc.dram_tensor("y", [T, D], F32, kind="ExternalOutput").ap()
        dbg_out = None
        if dbg is not None:
            dbg_out = nc.dram_tensor("dbg", list(dbg[0]), dbg[1], kind="ExternalOutput").ap()

        s_qgT = dscr("s_qgT", [256, T], BF16)
        s_kgT = dscr("s_kgT", [256, T], BF16)
        s_kg = dscr("s_kg", [T, 256], BF16)
        s_vg = dscr("s_vg", [T, 512], BF16)
        s_gg = dscr("s_gg", [T, 512], BF16)
        s_alrT = dscr("s_alrT", [32, T], BF16)
        s_qdT = dscr("s_qdT", [512, T], BF16)
        s_kdT = dscr("s_kdT", [512, T], BF16)
        s_vd = dscr("s_vd", [T, 512], BF16)
        s_cat = dscr("s_cat", [T, D], BF16)
        s_x1 = dscr("s_x1", [T, D], F32)
        s_x2 = dscr("s_x2", [T, D], F32)
        s_x3 = dscr("s_x3", [T, D], F32)
        s_attnT = dscr("s_attnT", [D, T], BF16)

        ident_b = kb.sbuf("ident_b", [128, 128], BF16)
        ident_f = kb.sbuf("ident_f", [128, 128], F32)
        ones_f = kb.sbuf("ones_f", [128, 128], F32)
        ones_b = kb.sbuf("ones_b", [128, 128], BF16)
        XA = kb.sbuf("XA", [128, 8, T], BF16)
        eps5 = kb.sbuf("eps5", [128, 1], F32)
        eps6 = kb.sbuf("eps6", [128, 1], F32)
        one1 = kb.sbuf("one1", [128, 1], F32)
        ln_stats = kb.sbuf("ln_stats", [128, NT, 2, 6], F32)
        ln_mv = kb.sbuf("ln_mv", [128, NT, 2], F32)
        ln_sc = kb.sbuf("ln_sc", [128, NT, 4], F32)
        bar_a = kb.sbuf("bar_a", [128, 8], F32)
        bar_v = kb.sbuf("bar_v", [128, 8], F32)
        bar_g = kb.sbuf("bar_g", [128, 8], F32)
        pds = [es.enter_context(nc.psum_tensor(f"pd{i}", [128, 1024], F32)) for i in range(4)]
        banks = [pds[i // 2][:, (i % 2) * 512:(i % 2 + 1) * 512] for i in range(8)]
        PSB = PSBanks(banks)

        PSB_T = PSBanks(banks[0:4])
        csem = kb.new_sem("csem")
        st_sem = kb.new_sem("st_sem")

        csem_g = kb.new_sem("csem_g")
        ctok0 = kb.dma("gpsimd", ident_b[:], I("c_ident")[:, :], csem_g)
        ctok = kb.dma("sync", ident_f[:], I("c_ident")[:, :], csem)
        kb.op("vector", lambda e: e.memset(ones_f[:], 1.0))
        kb.op("vector", lambda e: e.memset(ones_b[:], 1.0))
        kb.op("vector", lambda e: e.memset(eps5[:], 1e-5))
        kb.op("vector", lambda e: e.memset(one1[:], 1.0))
        ctok2 = kb.op("vector", lambda e: e.memset(eps6[:], 1e-6), sig=True)
        CW = [ctok0, ctok, ctok2]

        store_sems = [st_sem]

        def new_store_sem(name):
            s_ = kb.new_sem(name)
            store_sems.append(s_)
            return s_

        def store(out, in_, waits, sem=None):
            return kb.dma("sync", out, in_, sem if sem is not None else st_sem, waits=waits)

        def barrier():
            toks = [
                kb.op("scalar", lambda e: e.memzero(bar_a[:, 0:4]), sig=True),
                kb.op("vector", lambda e: e.memset(bar_v[:, 0:4], 0.0), sig=True),
                kb.op("gpsimd", lambda e: e.memset(bar_g[:, 0:4], 0.0), sig=True),
            ]
            for s_ in store_sems:
                if s_.v:
                    toks.append((s_, s_.v))
            for e in KB.ENGS:
                for tk in toks:
                    kb.wait(e, tk)

        evac_flip = [0]

        def evac(out, in_, waits, scale=None, func=None, eng=None):
            if func is not None:
                return kb.op("scalar", lambda e: e.activation(out=out, in_=in_, func=func,
                                                               scale=(1.0 if scale is None else scale)),
                             waits=waits, sig=True)
            if eng is None:
                evac_flip[0] ^= 1
                eng = "scalar" if evac_flip[0] else "vector"
            if eng == "scalar":
                if scale is None:
                    return kb.op("scalar", lambda e: e.copy(out=out, in_=in_), waits=waits, sig=True)
                return kb.op("scalar", lambda e: e.mul(out=out, in_=in_, mul=scale), waits=waits, sig=True)
            if scale is None:
                return kb.op("vector", lambda e: e.tensor_copy(out=out, in_=in_), waits=waits, sig=True)
            return kb.op("vector", lambda e: e.tensor_scalar(out=out, in0=in_, scalar1=scale, scalar2=None,
                                                              op0=ALU.mult), waits=waits, sig=True)

        def to_fm(src_b, t, dstT, nchunk, src_waits):
            toks = []
            for c0 in range(0, nchunk, 8):
                n = min(8, nchunk - c0)
                b, bank, w = PSB_T.get()
                pb = bank[:].bitcast(BF16)
                mt = None
                for c in range(n):
                    mt = kb.op("tensor", lambda e, c=c: e.transpose(
                        out=pb[:, c * 128:(c + 1) * 128], in_=src_b[:, (c0 + c) * 128:(c0 + c + 1) * 128],
                        identity=ident_b[:]), waits=list(w) + list(src_waits) + CW, sig=(c == n - 1))
                tk = evac(dstT[:, c0:c0 + n, t * 128:(t + 1) * 128],
                          pb[:, 0:n * 128].rearrange("p (c k) -> p c k", k=128), [mt])
                PSB_T.rel(b, tk)
                toks.append(tk)
            return toks

        def layer_norm_tile(t, y, gB, bB, out_f, waits):
            kb.op("vector", lambda e: e.bn_stats(out=ln_stats[:, t, 0, :], in_=y[:, 0:512]), waits=waits)
            tb = kb.op("vector", lambda e: e.bn_stats(out=ln_stats[:, t, 1, :], in_=y[:, 512:1024]), sig=True)
            tc_ = kb.op("vector", lambda e: e.bn_aggr(out=ln_mv[:, t, :],
                                                      in_=ln_stats[:, t, :, :].rearrange("p a s -> p (a s)")),
                        waits=[tb], sig=True)
            td = kb.op("scalar", lambda e: e.activation(out=ln_sc[:, t, 0:1], in_=ln_mv[:, t, 1:2], func=AF.Ln,
                                                        bias=eps5[:, 0:1]), waits=[tc_] + CW, sig=True)
            te = kb.op("scalar", lambda e: e.activation(out=ln_sc[:, t, 2:3], in_=ln_sc[:, t, 0:1], func=AF.Exp,
                                                        scale=-0.5), waits=[td], sig=True)
            tf = kb.op("vector", lambda e: e.scalar_tensor_tensor(out=ln_sc[:, t, 3:4], in0=ln_mv[:, t, 0:1],
                                                                   scalar=-1.0, in1=ln_sc[:, t, 2:3],
                                                                   op0=ALU.mult, op1=ALU.mult),
                       waits=[te], sig=True)
            tg = kb.op("scalar", lambda e: e.activation(out=out_f, in_=y, func=AF.Identity, bias=ln_sc[:, t, 3:4],
                                                        scale=ln_sc[:, t, 2:3]), waits=[tf], sig=True)
            th = kb.op("vector", lambda e: e.tensor_tensor(out=out_f, in0=out_f, in1=gB, op=ALU.mult),
                       waits=[tg], sig=True)
            return kb.op("vector", lambda e: e.tensor_tensor(out=out_f, in0=out_f, in1=bB, op=ALU.add),
                         waits=[th], sig=True)

        def load_ln_params(pes, li):
            sem = kb.new_sem(f"lnp{li}")
            gB = pes.enter_context(nc.sbuf_tensor(f"lngB{li}", [128, D], F32))
            bB = pes.enter_context(nc.sbuf_tensor(f"lnbB{li}", [128, D], F32))
            kb.dma("sync", gB[:], I("ln_g")[li, :].partition_broadcast(128), sem)
            tk = kb.dma("sync", bB[:], I("ln_b")[li, :].partition_broadcast(128), sem)
            return gB, bB, tk

        class LNRings:
            def __init__(self, pes, tag):
                mk = lambda nm, shp, dt, n: [pes.enter_context(nc.sbuf_tensor(f"{nm}{tag}{i}", shp, dt))
                                             for i in range(n)]
                self.xres = Ring(mk("lxr", [128, D], F32, 2))
                self.xres_sems = [kb.new_sem(f"lxrs{tag}{i}") for i in range(2)]
                self.y = Ring(mk("lyy", [128, D], F32, 2))
                self.out = Ring(mk("lout", [128, D], F32, 2))
                self.out_sems = [new_store_sem(f"louts{tag}{i}") for i in range(2)]
                self.b16 = Ring(mk("lb16", [128, D], BF16, 2))

        def resid_ln_tile(t, halves, xres_dram, gB, bB, gbtok, out_dram, out_XT, R):
            xb_, xres, xw = R.xres.get()
            lt = kb.dma("sync", xres[:], xres_dram[t * 128:(t + 1) * 128, :], R.xres_sems[xb_], waits=xw)
            yb_, y, yw = R.y.get()
            toks = []
            for dh, (src, sw) in enumerate(halves):
                tk = kb.op("vector", lambda e, dh=dh, src=src: e.scalar_tensor_tensor(
                    out=y[:, dh * 512:(dh + 1) * 512], in0=xres[:, dh * 512:(dh + 1) * 512], scalar=ALPHA, in1=src,
                    op0=ALU.mult, op1=ALU.add), waits=[lt] + list(sw) + list(yw), sig=True)
                toks.append(tk)
            R.xres.rel(xb_, toks[-1])
            ob_, o, ow = R.out.get()
            lt2 = layer_norm_tile(t, y[:], gB[:], bB[:], o[:], [toks[-1], gbtok] + list(ow))
            R.y.rel(yb_, lt2)
            stok = store(out_dram[t * 128:(t + 1) * 128, :], o[:], [lt2], R.out_sems[ob_])
            R.out.rel(ob_, stok)
            if out_XT is not None:
                bb_, ob16, bw = R.b16.get()
                ct = kb.op("scalar", lambda e: e.copy(out=ob16[:], in_=o[:]), waits=[lt2] + list(bw), sig=True)
                R.out.rel(ob_, ct)
                tks = to_fm(ob16, t, out_XT, 8, [ct])
                R.b16.rel(bb_, tks[-1])
            return toks

        def resid_ln_all(get_halves, ydst, xres_dram, gB, bB, gbtok, out_dram, out_XT, R, rel_cb=None):
            p1 = None
            for t in range(NT):
                halves = get_halves(t)
                xb_, xres, xw = R.xres.get()
                lt = kb.dma("sync", xres[:], xres_dram[t * 128:(t + 1) * 128, :], R.xres_sems[xb_], waits=xw)
                toks = []
                for dh, (src, sw) in enumerate(halves):
                    tk = kb.op("vector", lambda e, dh=dh, src=src: e.scalar_tensor_tensor(
                        out=ydst[:, t, dh * 512:(dh + 1) * 512], in0=xres[:, dh * 512:(dh + 1) * 512], scalar=ALPHA,
                        in1=src, op0=ALU.mult, op1=ALU.add), waits=[lt] + list(sw), sig=True)
                    toks.append(tk)
                R.xres.rel(xb_, toks[-1])
                if rel_cb is not None:
                    rel_cb(t, toks)
                kb.op("vector", lambda e: e.bn_stats(out=ln_stats[:, t, 0, :], in_=ydst[:, t, 0:512]), waits=toks)
                tb = kb.op("vector", lambda e: e.bn_stats(out=ln_stats[:, t, 1, :], in_=ydst[:, t, 512:1024]), sig=True)
                p1 = kb.op("vector", lambda e: e.bn_aggr(out=ln_mv[:, t, :],
                                                         in_=ln_stats[:, t, :, :].rearrange("p a s -> p (a s)")),
                           waits=[tb], sig=True)
            td = kb.op("scalar", lambda e: e.activation(out=ln_sc[:, :, 0], in_=ln_mv[:, :, 1], func=AF.Ln,
                                                        bias=eps5[:, 0:1]), waits=[p1] + CW, sig=True)
            te = kb.op("scalar", lambda e: e.activation(out=ln_sc[:, :, 2], in_=ln_sc[:, :, 0], func=AF.Exp,
                                                        scale=-0.5), waits=[td], sig=True)
            tf = kb.op("vector", lambda e: e.scalar_tensor_tensor(out=ln_sc[:, :, 3], in0=ln_mv[:, :, 0], scalar=-1.0,
                                                                   in1=ln_sc[:, :, 2], op0=ALU.mult, op1=ALU.mult),
                       waits=[te], sig=True)
            for t in range(NT):
                ob_, o, ow = R.out.get()
                tg = kb.op("scalar", lambda e: e.activation(out=o[:], in_=ydst[:, t, :], func=AF.Identity,
                                                            bias=ln_sc[:, t, 3:4], scale=ln_sc[:, t, 2:3]),
                           waits=[tf] + list(ow), sig=True)
                th = kb.op("vector", lambda e: e.tensor_tensor(out=o[:], in0=o[:], in1=gB[:], op=ALU.mult),
                           waits=[tg, gbtok], sig=True)
                ti = kb.op("vector", lambda e: e.tensor_tensor(out=o[:], in0=o[:], in1=bB[:], op=ALU.add),
                           waits=[th], sig=True)
                stok = store(out_dram[t * 128:(t + 1) * 128, :], o[:], [ti], R.out_sems[ob_])
                R.out.rel(ob_, stok)
                if out_XT is not None:
                    bb_, ob16, bw = R.b16.get()
                    ct = kb.op("scalar", lambda e: e.copy(out=ob16[:], in_=o[:]), waits=[ti] + list(bw), sig=True)
                    R.out.rel(ob_, ct)
                    tks = to_fm(ob16, t, out_XT, 8, [ct])
                    R.b16.rel(bb_, tks[-1])

        def finish():
            for s_ in store_sems:
                if s_.v:
                    kb.wait("sync", (s_, s_.v))

        gla_es = ExitStack()
        qgT = gla_es.enter_context(nc.sbuf_tensor("qgT", [128, 2, T], BF16))
        kgT = gla_es.enter_context(nc.sbuf_tensor("kgT", [128, 2, T], BF16))
        kgm = gla_es.enter_context(nc.sbuf_tensor("kgm", [128, NT, 256], BF16))
        vg = gla_es.enter_context(nc.sbuf_tensor("vg", [128, NT, 512], BF16))
        alrT = gla_es.enter_context(nc.sbuf_tensor("alrT", [32, T], BF16))
        wA_es = ExitStack()
        wA = wA_es.enter_context(nc.sbuf_tensor("wA", [128, 8, EVEN_IN], BF16))
        with ExitStack() as pes:
            xb_bufs = [pes.enter_context(nc.sbuf_tensor(f"xb{i}", [128, D], BF16)) for i in range(2)]
            xb_sems = [kb.new_sem(f"xbs{i}") for i in range(2)]
            ring = Ring(xb_bufs)
            for t in range(NT):
                b, xb, w = ring.get()
                lt = kb.dma("gpsimd", xb[:], I("x_in")[t * 128:(t + 1) * 128, :], xb_sems[b], waits=w)
                toks = to_fm(xb, t, XA, 8, [lt])
                ring.rel(b, toks[-1])
            wsem = kb.new_sem("wAsem")
            wv = I("w_in0").rearrange("(c p) n -> p c n", p=128)
            wtok = None
            for c in range(8):
                wtok = kb.dma("gpsimd", wA[:, c, :], wv[:, c, :], wsem)
            barrier()

        if stage == 0:
            store(dbg_out.rearrange("(c p) t -> p c t", p=128), XA[:], [])
            finish()
            nc.used_inputs = list(used_inputs)
            return nc

        with ExitStack() as pes:
            stg = Ring([pes.enter_context(nc.sbuf_tensor(f"stgA{i}", [128, 512], BF16)) for i in range(4)])
            stg_sems = [new_store_sem(f"stgAs{i}") for i in range(4)]
            fm_list = []
            for c in range(2):
                fm_list.append((c * 128, 128, None, 0.125, qgT[:, c, :]))
            for c in range(2):
                fm_list.append((256 + c * 128, 128, None, None, kgT[:, c, :]))
            fm_list.append((1536, 32, None, None, alrT[0:32, :]))
            for c in range(4):
                fm_list.append((1568 + c * 128, 128, s_qdT[c * 128:(c + 1) * 128, :], 0.125, None))
            for c in range(4):
                fm_list.append((2080 + c * 128, 128, s_kdT[c * 128:(c + 1) * 128, :], None, None))
            for (c0, M, dst, scale, res) in fm_list:
                for tt in range(4):
                    b, bank, w = PSB.get()
                    mt = None
                    for kc in range(8):
                        mt = kb.op("tensor", lambda e, kc=kc: e.matmul(
                            bank[0:M, :], lhsT=wA[:, kc, c0:c0 + M], rhs=XA[:, kc, tt * 512:(tt + 1) * 512],
                            start=(kc == 0), stop=(kc == 7)), waits=list(w) + [wtok], sig=(kc == 7))
                    if res is not None:
                        tk = evac(res[:, tt * 512:(tt + 1) * 512], bank[0:M, :], [mt], scale=scale)
                        PSB.rel(b, tk)
                        continue
                    sb, st, sw = stg.get()
                    tk = evac(st[0:M, :], bank[0:M, :], [mt] + list(sw), scale=scale)
                    PSB.rel(b, tk)
                    stok = store(dst[:, tt * 512:(tt + 1) * 512], st[0:M, :], [tk], stg_sems[sb])
                    stg.rel(sb, stok)
            tm_list = [(256, 256, None, None, kgm), (512, 512, None, None, vg), (1024, 512, s_gg, AF.Silu, None),
                       (2592, 512, s_vd, None, None)]
            for (c0, W, dst, func, res) in tm_list:
                for t in range(NT):
                    b, bank, w = PSB.get()
                    mt = None
                    for kc in range(8):
                        mt = kb.op("tensor", lambda e, kc=kc: e.matmul(
                            bank[:, 0:W], lhsT=XA[:, kc, t * 128:(t + 1) * 128], rhs=wA[:, kc, c0:c0 + W],
                            start=(kc == 0), stop=(kc == 7)), waits=list(w) + [wtok], sig=(kc == 7))
                    if res is not None:
                        tk = evac(res[:, t, :], bank[:, 0:W], [mt], func=func)
                        PSB.rel(b, tk)
                        continue
                    sb, st, sw = stg.get()
                    tk = evac(st[:, 0:W], bank[:, 0:W], [mt] + list(sw), func=func)
                    PSB.rel(b, tk)
                    stok = store(dst[t * 128:(t + 1) * 128, :], st[:, 0:W], [tk], stg_sems[sb])
                    stg.rel(sb, stok)
            barrier()
        wA_es.close()

        if stage == 1:
            with nc.sbuf_tensor("dbgt", [128, 16, 256], BF16) as dt_:
                sm = kb.new_sem("dbgs")
                tk = kb.dma("sync", dt_[:], s_kg.rearrange("(t p) n -> p t n", p=128), sm)
                store(dbg_out.rearrange("(t p) n -> p t n", p=128), dt_[:], [tk])
                finish()
            nc.used_inputs = list(used_inputs)
            return nc

        def dump_dram(view, shape3, dt):
            with nc.sbuf_tensor("dbgt", list(shape3), dt) as dt_:
                sm = kb.new_sem("dbgs")
                tk = kb.dma("sync", dt_[:], view, sm)
                return dt_, tk

        if True:
            with ExitStack() as pes:
                sb = lambda nm, shp, dt: pes.enter_context(nc.sbuf_tensor(nm, shp, dt))
                gbd = sb("gbd", [32, 512], BF16)
                gbs = sb("gbs", [1, 512], BF16)
                RF = sb("RF", [128, 130], F32)
                RB = sb("RB", [128, 130], F32)
                SU = sb("SU", [128, 128], F32)
                SL = sb("SL", [128, 128], F32)
                mFB = sb("mFB", [128, 256], F32)
                gainG = sb("gainG", [128, 128], F32)
                qf, kf, qb, kbb = XA[:, 0:2, :], XA[:, 2:4, :], XA[:, 4:6, :], XA[:, 6:8, :]
                ke = sb("ke", [128, NT, 512], BF16)
                dec = sb("dec", [128, 2, 2, 32], F32)
                S_all = sb("S_all", [128, 2, 2, 32, 128], BF16)
                S32 = sb("S32", [128, 2, 2, 2, 128], F32)
                catg = sb("catg", [128, NT, 512], BF16)
                ssg = sb("ssg", [128, NT, 4], F32)
                rsg = sb("rsg", [128, NT, 8], F32)
                e1r = Ring([sb(f"e1b{i}", [128, 512], F32) for i in range(2)])
                lpr = Ring([sb(f"lpb{i}", [128, 512], F32) for i in range(2)])
                gker = Ring([sb(f"gke{i}", [128, 512], F32) for i in range(2)])
                gtr = Ring([sb(f"gts{i}", [128, 2, 2, 2, 128], F32) for i in range(2)])
                amr = Ring([sb(f"am{i}", [128, 256], BF16) for i in range(8)])
                osr = Ring([sb(f"osb{i}", [128, 512], F32) for i in range(2)])
                tnr = Ring([sb(f"tnb{i}", [128, 512], F32) for i in range(2)])
                sgr_ = Ring([sb(f"sgb{i}", [128, 512], BF16) for i in range(2)])
                sg_sems = [kb.new_sem(f"sgs{i}") for i in range(2)]
                jg = sb("jg", [128, 4, 128], F32)
                jg_tok = [None] * 4
                ls = kb.new_sem("gla_s")
                lg = kb.new_sem("gla_g")
                kb.dma("sync", RF[:], I("c_RF")[:, :], ls)
                kb.dma("sync", RB[:], I("c_RB")[:, :], ls)
                kb.dma("sync", SU[:], I("c_SU")[:, :], ls)
                kb.dma("sync", SL[:], I("c_SL")[:, :], ls)
                kb.dma("sync", mFB[:, 0:128], I("c_RF")[:, 0:128], ls)
                kb.dma("sync", mFB[:, 128:256], I("c_RB")[:, 0:128], ls)
                ltok = kb.dma("sync", gainG[:], I("gla_gain")[0, :].partition_broadcast(128), ls)
                kb.dma("gpsimd", gbd[:], I("gate_bd")[:, :], lg)
                gtok = kb.dma("gpsimd", gbs[:], I("gate_bias")[:, :], lg)
                LW = [ltok, gtok]
                tz0 = kb.op("vector", lambda e: e.memset(S32[:].rearrange("p a b c d -> p (a b c d)"), 0.0), sig=True)
                tz1 = kb.op("gpsimd", lambda e: e.memset(S_all[:, 0, :, 0, :], 0.0), sig=True)
                tz2 = kb.op("gpsimd", lambda e: e.memset(S_all[:, 1, :, 31, :], 0.0), sig=True)
                PS1 = PSBanks(banks[0:8])
                p1_toks = []
                def gp_stage1(t):
                        sl = slice(t * 128, (t + 1) * 128)
                        bz, bankz, wz = PS1.get()
                        kb.op("tensor", lambda e: e.matmul(bankz[:, :], lhsT=alrT[0:32, sl], rhs=gbd[0:32, :], start=True,
                                                           stop=False), waits=list(wz) + LW + CW)
                        mz = kb.op("tensor", lambda e: e.matmul(bankz[:, :], lhsT=ones_b[0:1, 0:128], rhs=gbs[0:1, :],
                                                                start=False, stop=True), sig=True)
                        ei, e1, ew = e1r.get()
                        te1 = kb.op("scalar", lambda e: e.activation(out=e1[:], in_=bankz[:, :], func=AF.Exp, scale=-1.0),
                                    waits=[mz] + list(ew), sig=True)
                        PS1.rel(bz, te1)
                        li_, lp, lw = lpr.get()
                        tlp = kb.op("scalar", lambda e: e.activation(out=lp[:], in_=e1[:], func=AF.Ln, bias=one1[:, 0:1]),
                                    waits=[te1] + list(lw) + CW, sig=True)
                        e1r.rel(ei, tlp)
                        return (li_, lp, tlp)

                def gp_stage2(t, ctx):
                        li_, lp, tlp = ctx
                        sl = slice(t * 128, (t + 1) * 128)
                        bf_, bankF, wf = PS1.get()
                        bb_, bankB, wb = PS1.get()
                        be_, bankE, we = PS1.get()
                        mF = mB = mE = None
                        for p in range(2):
                            mF = kb.op("tensor", lambda e: e.matmul(bankF[:, p * 130:(p + 1) * 130],
                                                                    lhsT=lp[:, p * 128:(p + 1) * 128], rhs=RF[:, :],
                                                                    start=True, stop=True), waits=[tlp] + list(wf), sig=(p == 1))
                        for p in range(2):
                            mB = kb.op("tensor", lambda e: e.matmul(bankB[:, p * 130:(p + 1) * 130],
                                                                    lhsT=lp[:, 256 + p * 128:256 + (p + 1) * 128], rhs=RB[:, :],
                                                                    start=True, stop=True), waits=list(wb), sig=(p == 1))
                        kb.op("tensor", lambda e: e.matmul(bankE[:, 0:256], lhsT=SU[:, :], rhs=lp[:, 0:256], start=True,
                                                           stop=True), waits=list(we))
                        mE = kb.op("tensor", lambda e: e.matmul(bankE[:, 256:512], lhsT=SL[:, :], rhs=lp[:, 256:512],
                                                                start=True, stop=True), sig=True)
                        lpr.rel(li_, mE)
                        gi_, gts, gw = gtr.get()
                        vF = bankF[:, 0:260].rearrange("p (a c) -> p a c", c=130)
                        vB = bankB[:, 0:260].rearrange("p (a c) -> p a c", c=130)
                        kb.op("scalar", lambda e: e.activation(out=gts[:, 0, 0, :, :], in_=vF[:, :, 0:128], func=AF.Exp,
                                                               scale=-1.0 / 16), waits=[mF] + list(gw))
                        kb.op("scalar", lambda e: e.activation(out=gts[:, 0, 1, :, :], in_=vF[:, :, 0:128], func=AF.Exp,
                                                               scale=1.0 / 16))
                        tdF = kb.op("scalar", lambda e: e.activation(out=dec[:, 0, :, 2 * t:2 * t + 2], in_=vF[:, :, 128:130],
                                                                     func=AF.Exp, scale=-1.0 / 16), sig=True)
                        PS1.rel(bf_, tdF)
                        kb.op("scalar", lambda e: e.activation(out=gts[:, 1, 0, :, :], in_=vB[:, :, 0:128], func=AF.Exp,
                                                               scale=-1.0 / 16), waits=[mB])
                        kb.op("scalar", lambda e: e.activation(out=gts[:, 1, 1, :, :], in_=vB[:, :, 0:128], func=AF.Exp,
                                                               scale=1.0 / 16))
                        tdB = kb.op("scalar", lambda e: e.activation(out=dec[:, 1, :, 2 * t:2 * t + 2], in_=vB[:, :, 128:130],
                                                                     func=AF.Exp, scale=-1.0 / 16), sig=True)
                        PS1.rel(bb_, tdB)
                        ki_, gke, kw_ = gker.get()
                        tke = kb.op("scalar", lambda e: e.activation(out=gke[:], in_=bankE[:, :], func=AF.Exp,
                                                                     scale=-1.0 / 16), waits=[mE] + list(kw_), sig=True)
                        PS1.rel(be_, tke)
                        a1 = kb.op("vector", lambda e: e.tensor_tensor(out=qf[:, :, sl], in0=qgT[:, :, sl], in1=gts[:, 0, 0, :, :],
                                                                       op=ALU.mult), waits=[tdB] + LW, sig=True)
                        a2 = kb.op("vector", lambda e: e.tensor_tensor(out=qb[:, :, sl], in0=qgT[:, :, sl], in1=gts[:, 1, 0, :, :],
                                                                       op=ALU.mult), sig=True)
                        a3 = kb.op("vector", lambda e: e.tensor_tensor(out=ke[:, t, 0:256], in0=kgm[:, t, :], in1=gke[:, 0:256],
                                                                       op=ALU.mult), waits=[tke], sig=True)
                        g1 = kb.op("gpsimd", lambda e: e.tensor_tensor(out=kf[:, :, sl], in0=kgT[:, :, sl], in1=gts[:, 0, 1, :, :],
                                                                       op=ALU.mult), waits=[tdB] + LW, sig=True)
                        g2 = kb.op("gpsimd", lambda e: e.tensor_tensor(out=kbb[:, :, sl], in0=kgT[:, :, sl],
                                                                       in1=gts[:, 1, 1, :, :], op=ALU.mult), sig=True)
                        g3 = kb.op("gpsimd", lambda e: e.tensor_tensor(out=ke[:, t, 256:512], in0=kgm[:, t, :],
                                                                       in1=gke[:, 256:512], op=ALU.mult), waits=[tke], sig=True)
                        gtr.rel(gi_, a2)
                        gtr.rel(gi_, g2)
                        gker.rel(ki_, a3)
                        gker.rel(ki_, g3)
                        p1_toks = [a1, a2, a3, g1, g2, g3, tdF, tdB]
                        return [a1, a2, a3, g1, g2, g3, tdF, tdB]

                gp_ctx = {0: gp_stage1(0)}
                for t in range(NT):
                    if t + 1 < NT:
                        gp_ctx[t + 1] = gp_stage1(t + 1)
                    p1_toks = gp_stage2(t, gp_ctx[t])
                P1 = list(p1_toks)
                PS2 = PSBanks(banks[0:4])
                orders = [list(range(0, 31)), list(range(31, 0, -1))]
                prev_copy = [tz0, tz0]
                for step in range(31):
                    for d_ in range(2):
                        n = orders[d_][step]
                        t = n // 2
                        rows = slice((n % 2) * 64, (n % 2) * 64 + 64)
                        bk, bankK, wk = PS2.get()
                        mk = None
                        for p in range(2):
                            for hh in range(2):
                                h = p * 2 + hh
                                mk = kb.op("tensor", lambda e: e.matmul(
                                    bankK[hh * 64:(hh + 1) * 64, p * 128:(p + 1) * 128],
                                    lhsT=ke[rows, t, d_ * 256 + h * 64:d_ * 256 + (h + 1) * 64],
                                    rhs=vg[rows, t, h * 128:(h + 1) * 128], start=True, stop=True),
                                    waits=list(wk) + P1, sig=(h == 3))
                        cur, nxt = step % 2, (step + 1) % 2
                        ts_ = None
                        for p in range(2):
                            ts_ = kb.op("vector", lambda e: e.scalar_tensor_tensor(
                                out=S32[:, d_, nxt, p, :], in0=S32[:, d_, cur, p, :], scalar=dec[:, d_, p, n:n + 1],
                                in1=bankK[:, p * 128:(p + 1) * 128], op0=ALU.mult, op1=ALU.add),
                                waits=[mk, prev_copy[d_]] + P1, sig=True)
                        PS2.rel(bk, ts_)
                        dst_n = n + 1 if d_ == 0 else n - 1
                        prev_copy[d_] = kb.op("vector", lambda e: e.tensor_copy(out=S_all[:, d_, :, dst_n, :],
                                                                                in_=S32[:, d_, nxt, :, :]),
                                              waits=[ts_, tz1, tz2], sig=True)
                P1 = P1 + prev_copy
                PSA = PSBanks(banks[0:4])
                PSO2 = PSBanks(banks[4:8])
                tfin = None
                def g_stage1(t):
                    sl = slice(t * 128, (t + 1) * 128)
                    res = []
                    for h in range(4):
                        p, hh = h // 2, h % 2
                        r = slice(hh * 64, hh * 64 + 64)
                        ba, bankA, wa = PSA.get()
                        kb.op("tensor", lambda e: e.matmul(bankA[:, 0:128], lhsT=kf[r, p, sl], rhs=qf[r, p, sl], start=True,
                                                           stop=True), waits=list(wa) + P1)
                        ma = kb.op("tensor", lambda e: e.matmul(bankA[:, 128:256], lhsT=kbb[r, p, sl], rhs=qb[r, p, sl],
                                                                start=True, stop=True), sig=True)
                        ai, am, aw = amr.get()
                        tam = kb.op("vector", lambda e: e.tensor_tensor(out=am[:], in0=bankA[:, 0:256], in1=mFB[:],
                                                                        op=ALU.mult), waits=[ma] + list(aw) + LW, sig=True)
                        PSA.rel(ba, tam)
                        res.append((ai, am, tam))
                    return res

                g_ctx = {0: g_stage1(0)}
                for t in range(NT):
                    if t + 1 < NT:
                        g_ctx[t + 1] = g_stage1(t + 1)
                    sl = slice(t * 128, (t + 1) * 128)
                    si_, sgt, sw = sgr_.get()
                    lsg = kb.dma("sync", sgt[:], s_gg[t * 128:(t + 1) * 128, :], sg_sems[si_], waits=sw)
                    bo, banko, wo_ = PSO2.get()
                    mo = None
                    for h in range(4):
                        p, hh = h // 2, h % 2
                        r = slice(hh * 64, hh * 64 + 64)
                        ai, am, tam = g_ctx[t][h]
                        oh = banko[:, h * 128:(h + 1) * 128]
                        kb.op("tensor", lambda e: e.matmul(oh, lhsT=am[:, 0:128], rhs=vg[:, t, h * 128:(h + 1) * 128],
                                                           start=True, stop=False, skip_group_check=True),
                              waits=[tam] + (list(wo_) if h == 0 else []))
                        kb.op("tensor", lambda e: e.matmul(oh, lhsT=am[:, 128:256], rhs=vg[:, t, h * 128:(h + 1) * 128],
                                                           start=False, stop=False, skip_group_check=True))
                        for c in range(2):
                            cs = slice(t * 128 + c * 64, t * 128 + c * 64 + 64)
                            kb.op("tensor", lambda e: e.matmul(banko[c * 64:(c + 1) * 64, h * 128:(h + 1) * 128],
                                                               lhsT=qf[r, p, cs], rhs=S_all[r, 0, p, 2 * t + c, :],
                                                               start=False, stop=False, skip_group_check=True))
                        for c in range(2):
                            cs = slice(t * 128 + c * 64, t * 128 + c * 64 + 64)
                            mo = kb.op("tensor", lambda e: e.matmul(banko[c * 64:(c + 1) * 64, h * 128:(h + 1) * 128],
                                                                    lhsT=qb[r, p, cs], rhs=S_all[r, 1, p, 2 * t + c, :],
                                                                    start=False, stop=(c == 1), skip_group_check=True),
                                       sig=(c == 1))
                        amr.rel(ai, mo)
                    oi_, osb, ow = osr.get()
                    tcp = kb.op("scalar", lambda e: e.copy(out=osb[:], in_=banko[:, :]), waits=[mo] + list(ow), sig=True)
                    PSO2.rel(bo, tcp)
                    tss = None
                    for h in range(4):
                        tss = kb.op("vector", lambda e: e.scalar_tensor_tensor(
                            out=jg[:, h, :], in0=osb[:, h * 128:(h + 1) * 128], scalar=1.0, in1=osb[:, h * 128:(h + 1) * 128],
                            op0=ALU.mult, op1=ALU.mult, accum_out=ssg[:, t, h:h + 1]), waits=[tcp, jg_tok[h]], sig=True)
                        jg_tok[h] = tss
                    tln = kb.op("scalar", lambda e: e.activation(out=rsg[:, t, 0:4], in_=ssg[:, t, 0:4], func=AF.Ln,
                                                                 bias=eps6[:, 0:1], scale=1.0 / 128), waits=[tss] + CW,
                                sig=True)
                    tex = kb.op("scalar", lambda e: e.activation(out=rsg[:, t, 4:8], in_=rsg[:, t, 0:4], func=AF.Exp,
                                                                 scale=-0.5), waits=[tln], sig=True)
                    ni, tn_, nw = tnr.get()
                    tno = None
                    for h in range(4):
                        tno = kb.op("vector", lambda e: e.scalar_tensor_tensor(
                            out=tn_[:, h * 128:(h + 1) * 128], in0=osb[:, h * 128:(h + 1) * 128],
                            scalar=rsg[:, t, 4 + h:5 + h], in1=gainG[:], op0=ALU.mult, op1=ALU.mult),
                            waits=[tex] + list(nw) + LW, sig=True)
                    osr.rel(oi_, tno)
                    tfin = kb.op("gpsimd", lambda e: e.tensor_tensor(out=catg[:, t, :], in0=tn_[:], in1=sgt[:], op=ALU.mult),
                                 waits=[tno, lsg], sig=True)
                    tnr.rel(ni, tfin)
                    sgr_.rel(si_, tfin)
                store(s_cat.rearrange("(t p) n -> p t n", p=128)[:, :, 0:512], catg[:], [tfin])
                barrier()
        gla_es.close()

        with ExitStack() as pes:
            sb = lambda nm, shp, dt: pes.enter_context(nc.sbuf_tensor(nm, shp, dt))
            qd = sb("qd", [128, 4, T], BF16)
            kd = sb("kd", [128, 4, T], BF16)
            vaug = sb("vaugd", [128, NT, 4, 130], BF16)
            toe = sb("toe_sb", [128, 4, 6, 512], F32)
            far = sb("far_sb", [128, 8], F32)
            dl = sb("dl", [128, 256], F32)
            dlp = sb("dlp", [128, 128], F32)
            lam_s = sb("lam_s", [128, 8], F32)
            gainD = sb("gainD", [128, 128], F32)
            catd = sb("catd", [128, NT, 512], BF16)
            o0 = sb("o0", [128, 4, 128], F32)
            odr = Ring([sb(f"od{i}", [128, 4, 128], F32) for i in range(2)])
            junk = sb("junkd", [128, 4, 128], F32)
            junk_tok = [None] * 4
            rcs = sb("rcs", [128, 32, 16], F32)
            rstd_t = sb("rstd_t", [128, 16, 8], F32)
            ptr = Ring([sb(f"ptd{i}", [128, 512], BF16) for i in range(3)])
            tmpr = Ring([sb(f"tmpd{i}", [128, 512], F32) for i in range(2)])
            ls = kb.new_sem("dls")
            lg = kb.new_sem("dlg")
            kb.dma("sync", qd[:], s_qdT.rearrange("(c p) t -> p c t", p=128), ls)
            kb.dma("sync", kd[:], s_kdT.rearrange("(c p) t -> p c t", p=128), ls)
            for hh in range(4):
                kb.dma("sync", vaug[:, :, hh, 0:128],
                       s_vd.rearrange("(t p) n -> p t n", p=128)[:, :, hh * 128:(hh + 1) * 128], ls)
            kb.dma("sync", toe[:].rearrange("p a b c -> p (a b c)"), I("toe_in")[:, :], ls)
            kb.dma("sync", far[:], I("far_in")[0, :].partition_broadcast(128), ls)
            kb.dma("sync", dl[:], I("diff_lam")[0, :].partition_broadcast(128), ls)
            ldtok = kb.dma("sync", gainD[:], I("diff_gain")[0, :].partition_broadcast(128), ls)
            t_ones = kb.op("gpsimd", lambda e: e.memset(vaug[:, :, :, 128:129], 1.0), sig=True)
            t_g = kb.op("vector", lambda e: e.tensor_scalar(out=gainD[:], in0=gainD[:], scalar1=1.0 - LAM_INIT,
                                                            scalar2=None, op0=ALU.mult), waits=[ldtok], sig=True)
            t_a = kb.op("vector", lambda e: e.tensor_tensor(out=dlp[:, 0:64], in0=dl[:, 0:64], in1=dl[:, 64:128],
                                                            op=ALU.mult), waits=[ldtok], sig=True)
            t_b = kb.op("vector", lambda e: e.tensor_tensor(out=dlp[:, 64:128], in0=dl[:, 128:192], in1=dl[:, 192:256],
                                                            op=ALU.mult), sig=True)
            t_c = kb.op("vector", lambda e: e.tensor_reduce(out=lam_s[:, 0:2],
                                                            in_=dlp[:].rearrange("p (a d) -> p a d", a=2),
                                                            axis=mybir.AxisListType.X, op=ALU.add),
                        waits=[t_a, t_b], sig=True)
            t_d = kb.op("scalar", lambda e: e.activation(out=lam_s[:, 2:4], in_=lam_s[:, 0:2], func=AF.Exp),
                        waits=[t_c], sig=True)
            t_e = kb.op("vector", lambda e: e.tensor_tensor(out=lam_s[:, 4:5], in0=lam_s[:, 3:4], in1=lam_s[:, 2:3],
                                                            op=ALU.subtract), waits=[t_d], sig=True)
            t_lam = kb.op("vector", lambda e: e.tensor_scalar(out=lam_s[:, 5:6], in0=lam_s[:, 4:5], scalar1=-LAM_INIT,
                                                              scalar2=None, op0=ALU.add), waits=[t_e], sig=True)
            PS_S = PSBanks(banks[0:4])
            ACC = banks[4:8]
            acc_free = [[] for _ in range(4)]
            LOADW = [ldtok, t_ones]
            dctx = {"o0_tok": None, "tf": None}

            PS_D2 = PSBanks([pds[0], pds[1]])
            pt2d = Ring([sb(f"pt2d{i}", [128, 1024], BF16) for i in range(3)])
            tmp2r = Ring([sb(f"tmp2d{i}", [128, 1024], F32) for i in range(2)])

            def d_stage1(it):
                h, qt, m, kp = it
                b_, pd, w = PS_D2.get()
                mt = None
                for j in range(2):
                    kc = 2 * kp + j
                    mt = kb.op("tensor", lambda e: e.matmul(
                        pd[:, j * 512:(j + 1) * 512], lhsT=kd[m * 64:(m + 1) * 64, h, kc * 128:(kc + 1) * 128],
                        rhs=qd[m * 64:(m + 1) * 64, h, qt * 512:(qt + 1) * 512], start=True, stop=True),
                        waits=list(w) + LOADW, sig=(j == 1))
                pb_, PT, pw = pt2d.get()
                mrels = [2 * kp + j - 4 * qt for j in range(2)]
                near = [(-1 <= r <= 4) for r in mrels]
                if not any(near) and (mrels[0] > 0) == (mrels[1] > 0):
                    fi = h * 2 + (1 if mrels[0] > 0 else 0)
                    t2 = kb.op("scalar", lambda e: e.activation(out=PT[:], in_=pd[:, :], func=AF.Exp,
                                                                bias=far[:, fi:fi + 1]),
                               waits=[mt] + list(pw) + LOADW, sig=True)
                    PS_D2.rel(b_, t2)
                else:
                    tb_, tmp, tw = tmp2r.get()
                    t1 = None
                    for j in range(2):
                        if near[j]:
                            t1 = kb.op("vector", lambda e: e.tensor_tensor(
                                out=tmp[:, j * 512:(j + 1) * 512], in0=pd[:, j * 512:(j + 1) * 512],
                                in1=toe[:, h, mrels[j] + 1, :], op=ALU.add), waits=[mt] + list(tw) + LOADW, sig=True)
                        else:
                            fi = h * 2 + (1 if mrels[j] > 0 else 0)
                            t1 = kb.op("vector", lambda e: e.tensor_scalar(
                                out=tmp[:, j * 512:(j + 1) * 512], in0=pd[:, j * 512:(j + 1) * 512],
                                scalar1=far[:, fi:fi + 1], scalar2=None, op0=ALU.add),
                                waits=[mt] + list(tw) + LOADW, sig=True)
                    PS_D2.rel(b_, t1)
                    t2 = kb.op("scalar", lambda e: e.activation(out=PT[:], in_=tmp[:], func=AF.Exp),
                               waits=[t1] + list(pw), sig=True)
                    tmp2r.rel(tb_, t2)
                return (pb_, PT, t2)

            def d_stage2(it, ctx):
                h, qt, m, kp = it
                u = h * 4 + qt
                pb_, PT, t2 = ctx
                pv_last = None
                for j in range(2):
                    kc = 2 * kp + j
                    for qs in range(4):
                        w2 = [t2] + (acc_free[qs] if kc == 0 else [])
                        if kc == 0:
                            acc_free[qs] = []
                        pv_last = kb.op("tensor", lambda e: e.matmul(
                            ACC[qs][:, 0:129], lhsT=PT[:, j * 512 + qs * 128:j * 512 + (qs + 1) * 128],
                            rhs=vaug[:, kc, h, 0:129], start=(kc == 0), stop=(kc == NT - 1)), waits=w2,
                            sig=(qs == 3 and j == 1))
                pt2d.rel(pb_, pv_last)
                if kp != NT // 2 - 1:
                    return
                tr = None
                for qs in range(4):
                    tr = kb.op("vector", lambda e, qs=qs: e.reciprocal(out=rcs[:, u, m * 4 + qs:m * 4 + qs + 1],
                                                                        in_=ACC[qs][:, 128:129]),
                               waits=[pv_last], sig=(qs == 3))
                if m == 0:
                    tk = None
                    for qs in range(4):
                        tk = kb.op("vector", lambda e, qs=qs: e.tensor_scalar(
                            out=o0[:, qs, :], in0=ACC[qs][:, 0:128], scalar1=rcs[:, u, qs:qs + 1], scalar2=None,
                            op0=ALU.mult), waits=[tr, dctx["tf"]], sig=True)
                        acc_free[qs].append(tk)
                    dctx["o0_tok"] = tk
                else:
                    tl = kb.op("vector", lambda e: e.tensor_scalar(out=rcs[:, u, 8:12], in0=rcs[:, u, 4:8],
                                                                   scalar1=lam_s[:, 5:6], scalar2=None,
                                                                   op0=ALU.mult), waits=[tr, t_lam], sig=True)
                    ob_, od, ow = odr.get()
                    tod = None
                    for qs in range(4):
                        tod = kb.op("vector", lambda e, qs=qs: e.scalar_tensor_tensor(
                            out=od[:, qs, :], in0=ACC[qs][:, 0:128], scalar=rcs[:, u, 8 + qs:9 + qs],
                            in1=o0[:, qs, :], op0=ALU.mult, op1=ALU.add), waits=[tl, dctx["o0_tok"]] + list(ow),
                            sig=True)
                        acc_free[qs].append(tod)
                    tss = None
                    for qs in range(4):
                        tss = kb.op("vector", lambda e, qs=qs: e.scalar_tensor_tensor(
                            out=junk[:, qs, :], in0=od[:, qs, :], scalar=1.0, in1=od[:, qs, :], op0=ALU.mult,
                            op1=ALU.mult, accum_out=rcs[:, u, 12 + qs:13 + qs]), waits=[tod, junk_tok[qs]],
                            sig=True)
                        junk_tok[qs] = tss
                    tln = kb.op("scalar", lambda e: e.activation(out=rstd_t[:, u, 0:4], in_=rcs[:, u, 12:16],
                                                                 func=AF.Ln, bias=eps6[:, 0:1], scale=1.0 / 128),
                                waits=[tss] + CW, sig=True)
                    tex = kb.op("scalar", lambda e: e.activation(out=rstd_t[:, u, 4:8], in_=rstd_t[:, u, 0:4],
                                                                 func=AF.Exp, scale=-0.5), waits=[tln], sig=True)
                    tf_ = None
                    for qs in range(4):
                        tf_ = kb.op("vector", lambda e, qs=qs: e.scalar_tensor_tensor(
                            out=catd[:, qt * 4 + qs, h * 128:(h + 1) * 128], in0=od[:, qs, :],
                            scalar=rstd_t[:, u, 4 + qs:5 + qs], in1=gainD[:], op0=ALU.mult, op1=ALU.mult),
                            waits=[tex, t_g], sig=True)
                    odr.rel(ob_, tf_)
                    dctx["tf"] = tf_

            ditems = [(h, qt, m, kp) for h in range(4) for qt in range(4) for m in range(2) for kp in range(NT // 2)]
            prev = None
            for it in ditems:
                ctx = d_stage1(it)
                if prev is not None:
                    d_stage2(*prev)
                prev = (it, ctx)
            d_stage2(*prev)
            tf_ = dctx["tf"]
            store(s_cat.rearrange("(t p) n -> p t n", p=128)[:, :, 512:1024], catd[:], [tf_])
            barrier()

        if stage == 2:
            with nc.sbuf_tensor("dbgt2", [128, NT, D], BF16) as dt_:
                sm = kb.new_sem("dbgs2")
                tk = kb.dma("sync", dt_[:], s_cat.rearrange("(t p) n -> p t n", p=128), sm)
                store(dbg_out.rearrange("(t p) n -> p t n", p=128), dt_[:], [tk])
                finish()
            nc.used_inputs = list(used_inputs)
            return nc

        def outproj_ln(w_dram, src_dram_tm, src_dram_fm, xres_dram, li, out_dram, tag):
            with ExitStack() as pes:
                XB = pes.enter_context(nc.sbuf_tensor(f"XB{tag}", [128, 8, T], BF16))
                wo = pes.enter_context(nc.sbuf_tensor(f"wo{tag}", [128, 8, D], BF16))
                wsem = kb.new_sem(f"wos{tag}")
                wv = w_dram.rearrange("(c p) n -> p c n", p=128)
                wtok = None
                for c in range(8):
                    wtok = kb.dma("gpsimd", wo[:, c, :], wv[:, c, :], wsem)
                gB, bB, gbtok = load_ln_params(pes, li)
                R = LNRings(pes, tag)
                xready = []
                xtile = {}
                if src_dram_tm is not None:
                    cbufs = [pes.enter_context(nc.sbuf_tensor(f"cb{tag}{i}", [128, D], BF16)) for i in range(2)]
                    csems = [kb.new_sem(f"cbs{tag}{i}") for i in range(2)]
                    cr = Ring(cbufs)
                    for t in range(NT):
                        b_, cb, w = cr.get()
                        lt = kb.dma("sync", cb[:], src_dram_tm[t * 128:(t + 1) * 128, :], csems[b_], waits=w)
                        tks = to_fm(cb, t, XB, 8, [lt])
                        cr.rel(b_, tks[-1])
                        xtile[t] = tks
                else:
                    fview = src_dram_fm.rearrange("(c p) t -> p c t", p=128)
                    for q4 in range(4):
                        fsem = kb.new_sem(f"fms{tag}{q4}")
                        ftk = kb.dma("sync", XB[:, :, q4 * 512:(q4 + 1) * 512], fview[:, :, q4 * 512:(q4 + 1) * 512], fsem)
                        for t4 in range(4):
                            xtile[q4 * 4 + t4] = [ftk]
                PSO = PSBanks(banks[4:8])
                ybuf = pes.enter_context(nc.sbuf_tensor(f"ybuf{tag}", [128, NT, D], F32))
                rels = {}

                def get_halves(t):
                    halves = []
                    rels[t] = []
                    for dh in range(2):
                        b_, bank, w = PSO.get()
                        mt = None
                        for kc in range(8):
                            mt = kb.op("tensor", lambda e, kc=kc: e.matmul(
                                bank[:, :], lhsT=XB[:, kc, t * 128:(t + 1) * 128], rhs=wo[:, kc, dh * 512:(dh + 1) * 512],
                                start=(kc == 0), stop=(kc == 7)),
                                waits=list(w) + [wtok] + list(xready) + list(xtile.get(t, [])), sig=(kc == 7))
                        halves.append((bank[:, :], [mt]))
                        rels[t].append(b_)
                    return halves

                def rel_cb(t, toks):
                    for b_, tk in zip(rels[t], toks):
                        PSO.rel(b_, tk)

                resid_ln_all(get_halves, ybuf, xres_dram, gB, bB, gbtok, out_dram, XA, R, rel_cb)
                barrier()

        outproj_ln(I("w_out0"), s_cat, None, I("x_in"), 0, s_x1, "d0")

        if stage == 3:
            with nc.sbuf_tensor("dbgt3", [128, NT, D], F32) as dt_:
                sm = kb.new_sem("dbgs3")
                tk = kb.dma("sync", dt_[:], s_x1.rearrange("(t p) n -> p t n", p=128), sm)
                store(dbg_out.rearrange("(t p) n -> p t n", p=128), dt_[:], [tk])
                finish()
            nc.used_inputs = list(used_inputs)
            return nc

        def ffn_ln(experts, comb, xres_dram, li, out_dram, out_XT, tag, pre_compute=None):
            with ExitStack() as pes:
                sb = lambda nm, shp, dt: pes.enter_context(nc.sbuf_tensor(nm + tag, shp, dt))
                acc = sb("acc", [128, NT, D], F32)
                hTb = [sb(f"hT{i}", [128, 4, T], BF16) for i in range(2)]
                with ExitStack() as wes:
                    wsb = lambda nm, shp, dt: wes.enter_context(nc.sbuf_tensor(nm + tag, shp, dt))
                    wgb = [wsb(f"wg{i}", [128, 8, 512], BF16) for i in range(2)]
                    wub = [wsb(f"wu{i}", [128, 8, 512], BF16) for i in range(2)]
                    wdb = [wsb(f"wd{i}", [128, 4, D], BF16) for i in range(2)]
                    sgr = Ring([wsb(f"sg{i}", [128, 512], F32) for i in range(2)])
                    sems_gu = [kb.new_sem(f"ffgu{tag}{i}") for i in range(2)]
                    sems_d = [kb.new_sem(f"ffd{tag}{i}") for i in range(2)]
                    free_gu = [[], []]
                    free_d = [[], []]
                    items = [(ex, gi) for ex in experts for gi in range(len(FGROUPS))]
                    n_items = len(items)

                    def load_gu(idx):
                        (wg, wu, wd, e_), gi = items[idx]
                        f0, f1 = FGROUPS[gi]
                        nf = f1 - f0
                        s_ = idx % 2
                        w = free_gu[s_]
                        free_gu[s_] = []
                        kb.dma("gpsimd", wgb[s_][:, :, 0:nf * 128],
                               wg.rearrange("(c p) n -> p c n", p=128)[:, :, f0 * 128:f1 * 128], sems_gu[s_], waits=w)
                        return kb.dma("gpsimd", wub[s_][:, :, 0:nf * 128],
                                      wu.rearrange("(c p) n -> p c n", p=128)[:, :, f0 * 128:f1 * 128], sems_gu[s_])

                    def load_d(idx):
                        (wg, wu, wd, e_), gi = items[idx]
                        f0, f1 = FGROUPS[gi]
                        nf = f1 - f0
                        s_ = idx % 2
                        w = free_d[s_]
                        free_d[s_] = []
                        return kb.dma("gpsimd", wdb[s_][:, 0:nf, :],
                                      wd[f0 * 128:f1 * 128, :].rearrange("(c p) n -> p c n", p=128), sems_d[s_], waits=w)

                    PSG = PSBanks(banks[0:4])
                    PSO = PSBanks(banks[4:8])
                    acc_tok = [[None, None] for _ in range(NT)]
                    lg = {}
                    ld = {}

                    def emit_GU(idx):
                        (wg, wu, wd, e_), gi = items[idx]
                        f0, f1 = FGROUPS[gi]
                        nf = f1 - f0
                        s_ = idx % 2
                        hT = hTb[idx % 2]
                        hlast = None
                        mlast = None
                        for fc in range(nf):
                            for tt in range(4):
                                bg, bankg, wg_ = PSG.get()
                                bu, banku, wu_ = PSG.get()
                                mg = mu = None
                                for kc in range(8):
                                    mg = kb.op("tensor", lambda e, kc=kc: e.matmul(
                                        bankg[:, :], lhsT=wgb[s_][:, kc, fc * 128:(fc + 1) * 128],
                                        rhs=XA[:, kc, tt * 512:(tt + 1) * 512], start=(kc == 0), stop=(kc == 7)),
                                        waits=list(wg_) + [lg[idx]], sig=(kc == 7))
                                for kc in range(8):
                                    mu = kb.op("tensor", lambda e, kc=kc: e.matmul(
                                        banku[:, :], lhsT=wub[s_][:, kc, fc * 128:(fc + 1) * 128],
                                        rhs=XA[:, kc, tt * 512:(tt + 1) * 512], start=(kc == 0), stop=(kc == 7)),
                                        waits=list(wu_), sig=(kc == 7))
                                mlast = mu
                                sb_, sg, sw = sgr.get()
                                ta = kb.op("scalar", lambda e: e.activation(out=sg[:], in_=bankg[:, :], func=AF.Silu),
                                           waits=[mg] + list(sw), sig=True)
                                PSG.rel(bg, ta)
                                hlast = kb.op("vector", lambda e: e.tensor_tensor(
                                    out=hT[:, fc, tt * 512:(tt + 1) * 512], in0=sg[:], in1=banku[:, :], op=ALU.mult),
                                    waits=[ta, mu], sig=True)
                                PSG.rel(bu, hlast)
                                sgr.rel(sb_, hlast)
                        free_gu[s_].append(mlast)
                        return hlast

                    def emit_D(idx, hlast):
                        (wg, wu, wd, e_), gi = items[idx]
                        f0, f1 = FGROUPS[gi]
                        nf = f1 - f0
                        s_ = idx % 2
                        hT = hTb[idx % 2]
                        dlast = None
                        for t in range(NT):
                            for dh in range(2):
                                bo, banko, wo_ = PSO.get()
                                for fc in range(nf):
                                    dlast = kb.op("tensor", lambda e, fc=fc: e.matmul(
                                        banko[:, :], lhsT=hT[:, fc, t * 128:(t + 1) * 128],
                                        rhs=wdb[s_][:, fc, dh * 512:(dh + 1) * 512], start=(fc == 0), stop=(fc == nf - 1)),
                                        waits=list(wo_) + [hlast, ld[idx]], sig=(fc == nf - 1))
                                a_ap = acc[:, t, dh * 512:(dh + 1) * 512]
                                prev = acc_tok[t][dh]
                                if prev is None:
                                    if comb is None:
                                        tk = kb.op("vector", lambda e: e.tensor_copy(out=a_ap, in_=banko[:, :]),
                                                   waits=[dlast], sig=True)
                                    else:
                                        tk = kb.op("vector", lambda e: e.tensor_scalar(
                                            out=a_ap, in0=banko[:, :], scalar1=comb[:, t, e_:e_ + 1], scalar2=None,
                                            op0=ALU.mult), waits=[dlast, pre_tok], sig=True)
                                else:
                                    if comb is None:
                                        tk = kb.op("vector", lambda e: e.tensor_tensor(out=a_ap, in0=banko[:, :], in1=a_ap,
                                                                                       op=ALU.add),
                                                   waits=[dlast, prev], sig=True)
                                    else:
                                        tk = kb.op("vector", lambda e: e.scalar_tensor_tensor(
                                            out=a_ap, in0=banko[:, :], scalar=comb[:, t, e_:e_ + 1], in1=a_ap,
                                            op0=ALU.mult, op1=ALU.add), waits=[dlast, prev], sig=True)
                                acc_tok[t][dh] = tk
                                PSO.rel(bo, tk)
                        free_d[s_].append(dlast)

                    lg[0] = load_gu(0)
                    ld[0] = load_d(0)
                    if n_items > 1:
                        lg[1] = load_gu(1)
                        ld[1] = load_d(1)
                    pre_tok = pre_compute(acc) if pre_compute is not None else None
                    hl = {0: emit_GU(0)}
                    for idx in range(n_items):
                        if idx + 1 < n_items:
                            hl[idx + 1] = emit_GU(idx + 1)
                        if idx + 2 < n_items:
                            lg[idx + 2] = load_gu(idx + 2)
                        emit_D(idx, hl[idx])
                        if idx + 2 < n_items:
                            ld[idx + 2] = load_d(idx + 2)
                    barrier()
                gB, bB, gbtok = load_ln_params(pes, li)
                R = LNRings(pes, tag)
                resid_ln_all(lambda t: [(acc[:, t, dh * 512:(dh + 1) * 512], [acc_tok[t][dh]]) for dh in range(2)],
                             acc, xres_dram, gB, bB, gbtok, out_dram, out_XT, R)
                barrier()

        ffn_ln([(I("ffn_wg"), I("ffn_wu"), I("ffn_wd"), None)], None, s_x1, 1, s_x2, XA, "f0")

        if stage == 4:
            with nc.sbuf_tensor("dbgt4", [128, NT, D], F32) as dt_:
                sm = kb.new_sem("dbgs4")
                tk = kb.dma("sync", dt_[:], s_x2.rearrange("(t p) n -> p t n", p=128), sm)
                store(dbg_out.rearrange("(t p) n -> p t n", p=128), dt_[:], [tk])
                finish()
            nc.used_inputs = list(used_inputs)
            return nc

        with ExitStack() as pes:
            sb = lambda nm, shp, dt: pes.enter_context(nc.sbuf_tensor(nm, shp, dt))
            w1 = sb("w1", [128, 8, ODD_IN], BF16)
            w1r = sb("w1r", [128, 8, 32], BF16)
            gq = sb("gq", [128, 384], F32)
            cT = sb("cT", [128, 3, T], BF16)
            cosF = sb("cosF", [96, T], F32)
            sinF = sb("sinF", [96, T], F32)
            KR = sb("KR", [96, T], BF16)
            wuq = sb("wuq", [128, 2, 1536], BF16)
            wuqr = sb("wuqr", [128, 2, 16, 96], BF16)
            wukv = sb("wukv", [128, 16, 128], BF16)
            vaug = sb("vaugm", [128, NT, 16, 66], BF16)
            ssq = sb("ssq", [128, NT, 4], F32)
            rsq = sb("rsq", [128, NT, 4], F32)
            gsem = kb.new_sem("mla_g")
            ssem = kb.new_sem("mla_s")
            for c in range(8):
                kb.dma("gpsimd", w1[:, c, :], I("w_in1").rearrange("(c p) n -> p c n", p=128)[:, c, :], gsem)
            kb.dma("gpsimd", w1r[:], I("w_in1_rot").rearrange("(c p) n -> p c n", p=128), gsem)
            kb.dma("gpsimd", wuq[:], I("w_uq").rearrange("(c p) n -> p c n", p=128), gsem)
            tz = kb.op("vector", lambda e: e.memset(wuqr[:].rearrange("p a h r -> p (a h r)"), 0.0), sig=True)
            for c in range(2):
                kb.dma("gpsimd", wuqr[:, c, :, 64:96],
                       I("w_uq_rot").rearrange("(c p) (h r) -> p c h r", p=128, r=32)[:, c, :, :], gsem,
                       waits=[tz])
            gtok = kb.dma("gpsimd", wukv[:].rearrange("p h d -> p (h d)"), I("w_ukv")[:, :], gsem)
            kb.dma("sync", gq[:, 0:256], I("qn_gain")[0, :].partition_broadcast(128), ssem)
            kb.dma("sync", gq[:, 256:384], I("kvn_gain")[0, :].partition_broadcast(128), ssem)
            kb.dma("sync", cosF[:], I("c_cosF")[:, :], ssem)
            stok_ = kb.dma("sync", sinF[:], I("c_sinF")[:, :], ssem)
            t_ones = kb.op("gpsimd", lambda e: e.memset(vaug[:, :, :, 64:65], 1.0), sig=True)
            LW = [gtok, stok_]
            PS_P = PSBanks(banks[6:8])
            csb_all = sb("csb_all", [128, NT, 384], F32)
            cnr = Ring([sb(f"cnb{i}", [128, 384], BF16) for i in range(2)])
            jk = sb("jkm", [128, 2, 256], F32)
            jk_tok = [None, None]
            for t in range(NT):
                b_, bank, w = PS_P.get()
                mt = None
                for kc in range(8):
                    mt = kb.op("tensor", lambda e, kc=kc: e.matmul(
                        bank[:, 0:384], lhsT=XA[:, kc, t * 128:(t + 1) * 128], rhs=w1[:, kc, 0:384],
                        start=(kc == 0), stop=(kc == 7)), waits=list(w) + LW, sig=(kc == 7))
                tcp = kb.op("scalar", lambda e: e.copy(out=csb_all[:, t, :], in_=bank[:, 0:384]), waits=[mt], sig=True)
                PS_P.rel(b_, tcp)
                ta = kb.op("vector", lambda e: e.scalar_tensor_tensor(
                    out=jk[:, 0, :], in0=csb_all[:, t, 0:256], scalar=1.0, in1=csb_all[:, t, 0:256], op0=ALU.mult,
                    op1=ALU.mult, accum_out=ssq[:, t, 0:1]), waits=[tcp, jk_tok[0]], sig=True)
                jk_tok[0] = ta
                tb = kb.op("vector", lambda e: e.scalar_tensor_tensor(
                    out=jk[:, 1, 0:128], in0=csb_all[:, t, 256:384], scalar=1.0, in1=csb_all[:, t, 256:384],
                    op0=ALU.mult, op1=ALU.mult, accum_out=ssq[:, t, 1:2]), waits=[tcp, jk_tok[1]], sig=True)
                jk_tok[1] = tb
            tl1 = kb.op("scalar", lambda e: e.activation(out=rsq[:, :, 0], in_=ssq[:, :, 0], func=AF.Ln,
                                                         bias=eps6[:, 0:1], scale=1.0 / 256), waits=[ta, tb] + CW, sig=True)
            tl2 = kb.op("scalar", lambda e: e.activation(out=rsq[:, :, 1], in_=ssq[:, :, 1], func=AF.Ln,
                                                         bias=eps6[:, 0:1], scale=1.0 / 128), waits=[tl1], sig=True)
            te_ = kb.op("scalar", lambda e: e.activation(out=rsq[:, :, 2:4], in_=rsq[:, :, 0:2], func=AF.Exp,
                                                         scale=-0.5), waits=[tl2], sig=True)
            for t in range(NT):
                nb_, cn, nw = cnr.get()
                kb.op("vector", lambda e: e.scalar_tensor_tensor(
                    out=cn[:, 0:256], in0=csb_all[:, t, 0:256], scalar=rsq[:, t, 2:3], in1=gq[:, 0:256], op0=ALU.mult,
                    op1=ALU.mult), waits=[te_] + list(nw) + LW)
                tn = kb.op("vector", lambda e: e.scalar_tensor_tensor(
                    out=cn[:, 256:384], in0=csb_all[:, t, 256:384], scalar=rsq[:, t, 3:4], in1=gq[:, 256:384],
                    op0=ALU.mult, op1=ALU.mult), sig=True)
                tks_prev = tks if t > 0 else []
                tks = to_fm(cn, t, cT, 3, [tn])
                cnr.rel(nb_, tks[-1])
            cT_ready = list(tks_prev) + list(tks)
            t1r = Ring([sb(f"t1m{i}", [96, 512], F32) for i in range(2)])
            t2r = Ring([sb(f"t2m{i}", [96, 512], F32) for i in range(2)])
            kr_tok = None
            for tt in range(4):
                ba, bankA, wa = PS_P.get()
                bb, bankB, wb = PS_P.get()
                ma = mb_ = None
                for kc in range(8):
                    ma = kb.op("tensor", lambda e, kc=kc: e.matmul(
                        bankA[64:96, :], lhsT=w1[:, kc, 384:416], rhs=XA[:, kc, tt * 512:(tt + 1) * 512],
                        start=(kc == 0), stop=(kc == 7)), waits=list(wa) + LW, sig=(kc == 7))
                for kc in range(8):
                    mb_ = kb.op("tensor", lambda e, kc=kc: e.matmul(
                        bankB[64:96, :], lhsT=w1r[:, kc, 0:32], rhs=XA[:, kc, tt * 512:(tt + 1) * 512],
                        start=(kc == 0), stop=(kc == 7)), waits=list(wb), sig=(kc == 7))
                i1, t1, w1_ = t1r.get()
                i2, t2, w2_ = t2r.get()
                ka = kb.op("vector", lambda e: e.tensor_tensor(out=t1[64:96, :], in0=bankA[64:96, :],
                                                               in1=cosF[64:96, tt * 512:(tt + 1) * 512], op=ALU.mult),
                           waits=[ma] + list(w1_) + LW, sig=True)
                PS_P.rel(ba, ka)
                kbk = kb.op("vector", lambda e: e.tensor_tensor(out=t2[64:96, :], in0=bankB[64:96, :],
                                                                in1=sinF[64:96, tt * 512:(tt + 1) * 512], op=ALU.mult),
                            waits=[mb_] + list(w2_), sig=True)
                PS_P.rel(bb, kbk)
                kr_tok = kb.op("gpsimd", lambda e: e.tensor_tensor(out=KR[64:96, tt * 512:(tt + 1) * 512],
                                                                   in0=t1[64:96, :], in1=t2[64:96, :], op=ALU.add),
                               waits=[ka, kbk], sig=True)
                t1r.rel(i1, kr_tok)
                t2r.rel(i2, kr_tok)
            v_tok = None
            for t in range(NT):
                for hf in range(2):
                    b_, bank, w = PS_P.get()
                    mt = kb.op("tensor", lambda e: e.matmul(
                        bank[:, :], lhsT=cT[:, 2, t * 128:(t + 1) * 128], rhs=wukv[:, hf * 8:(hf + 1) * 8, 64:128],
                        start=True, stop=True), waits=list(w) + LW + list(cT_ready), sig=True)
                    v_tok = evac(vaug[:, t, hf * 8:(hf + 1) * 8, 0:64],
                                 bank[:, :].rearrange("p (h d) -> p h d", d=64), [mt, t_ones])
                    PS_P.rel(b_, v_tok)
            PS_O = PSBanks(banks[4:6])
            QTr = Ring([sb(f"QT{i}", [96, T], BF16) for i in range(2)])
            KTr = Ring([sb(f"KT{i}", [96, T], BF16) for i in range(2)])
            ptr = Ring([sb(f"ptm{i}", [128, 512], BF16) for i in range(3)])
            recr = Ring([sb(f"rec{i}", [65, 512], F32) for i in range(2)])
            bcr = Ring([sb(f"bcs{i}", [64, 512], F32) for i in range(2)])
            onr = Ring([sb(f"on{i}", [64, 512], BF16) for i in range(2)])
            on_sems = [new_store_sem(f"ons{i}") for i in range(2)]
            SC = 96 ** -0.5
            head_ctx = {}

            def mla_prep(h):
                qi, QT, qw = QTr.get()
                ki, KT, kw = KTr.get()
                qk_toks = []
                for tt in range(4):
                    ba, bankA, wa = PS_P.get()
                    bb, bankB, wb = PS_P.get()
                    ma = mb_ = None
                    for kc in range(2):
                        ma = kb.op("tensor", lambda e, kc=kc: e.matmul(
                            bankA[0:96, :], lhsT=wuq[:, kc, h * 96:(h + 1) * 96], rhs=cT[:, kc, tt * 512:(tt + 1) * 512],
                            start=(kc == 0), stop=(kc == 1)), waits=list(wa) + LW + list(cT_ready), sig=(kc == 1))
                    for kc in range(2):
                        mb_ = kb.op("tensor", lambda e, kc=kc: e.matmul(
                            bankB[0:96, :], lhsT=wuqr[:, kc, h, :], rhs=cT[:, kc, tt * 512:(tt + 1) * 512],
                            start=(kc == 0), stop=(kc == 1)), waits=list(wb), sig=(kc == 1))
                    i1, t1, w1_ = t1r.get()
                    i2, t2, w2_ = t2r.get()
                    ka = kb.op("vector", lambda e: e.tensor_tensor(out=t1[:, :], in0=bankA[0:96, :],
                                                                   in1=cosF[:, tt * 512:(tt + 1) * 512], op=ALU.mult),
                               waits=[ma] + list(w1_), sig=True)
                    PS_P.rel(ba, ka)
                    kbk = kb.op("vector", lambda e: e.tensor_tensor(out=t2[:, :], in0=bankB[0:96, :],
                                                                    in1=sinF[:, tt * 512:(tt + 1) * 512], op=ALU.mult),
                                waits=[mb_] + list(w2_), sig=True)
                    PS_P.rel(bb, kbk)
                    tq = kb.op("gpsimd", lambda e: e.tensor_tensor(out=QT[:, tt * 512:(tt + 1) * 512], in0=t1[:, :],
                                                                   in1=t2[:, :], op=ALU.add),
                               waits=[ka, kbk] + list(qw), sig=True)
                    t1r.rel(i1, tq)
                    t2r.rel(i2, tq)
                    bk, bankK, wk = PS_P.get()
                    mk = kb.op("tensor", lambda e: e.matmul(
                        bankK[0:64, :], lhsT=wukv[:, h, 0:64], rhs=cT[:, 2, tt * 512:(tt + 1) * 512],
                        start=True, stop=True), waits=list(wk) + LW + list(cT_ready), sig=True)
                    tk_ = kb.op("vector", lambda e: e.tensor_copy(out=KT[0:64, tt * 512:(tt + 1) * 512],
                                                                  in_=bankK[0:64, :]), waits=[mk] + list(kw), sig=True)
                    PS_P.rel(bk, tk_)
                    qk_toks += [tq, tk_]
                tkr = kb.op("gpsimd", lambda e: e.tensor_copy(out=KT[64:96, :], in_=KR[64:96, :]),
                            waits=[kr_tok] + list(kw), sig=True)
                qk_toks.append(tkr)
                head_ctx[h] = (qi, QT, ki, KT, qk_toks)

            deferred = []
            po_ctx = {}

            def flush_deferred():
                while deferred:
                    deferred.pop(0)()

            PS_S2 = PSBanks([pds[0], pds[1]])
            pt2r = Ring([sb(f"pt2m{i}", [128, 1024], BF16) for i in range(3)])

            def mla_stage1(it):
                h, qt, kp = it
                if qt == 0 and kp == 0 and h not in head_ctx:
                    mla_prep(h)
                if qt == 1 and kp == 0 and h + 1 < 16:
                    mla_prep(h + 1)
                if kp == 5:
                    flush_deferred()
                qi, QT, ki, KT, qk_toks = head_ctx[h]
                b_, pd, w = PS_S2.get()
                mt = None
                for j in range(2):
                    kc = 2 * kp + j
                    mt = kb.op("tensor", lambda e: e.matmul(
                        pd[:, j * 512:(j + 1) * 512], lhsT=KT[0:96, kc * 128:(kc + 1) * 128],
                        rhs=QT[0:96, qt * 512:(qt + 1) * 512], start=True, stop=True),
                        waits=list(w) + qk_toks, sig=(j == 1))
                pb_, PT, pw = pt2r.get()
                t2_ = kb.op("scalar", lambda e: e.activation(out=PT[:], in_=pd[:, :], func=AF.Exp, scale=SC),
                            waits=[mt] + list(pw), sig=True)
                PS_S2.rel(b_, t2_)
                return (pb_, PT, t2_)

            def mla_stage2(it, ctx):
                h, qt, kp = it
                pb_, PT, t2_ = ctx
                if kp == 0:
                    po_ctx[(h, qt)] = PS_O.get()
                bo, po, wo_ = po_ctx[(h, qt)]
                pv = None
                for j in range(2):
                    kc = 2 * kp + j
                    pv = kb.op("tensor", lambda e: e.matmul(
                        po[0:65, :], lhsT=vaug[:, kc, h, 0:65], rhs=PT[:, j * 512:(j + 1) * 512], start=(kc == 0),
                        stop=(kc == NT - 1)), waits=[t2_, v_tok] + (list(wo_) if kc == 0 else []), sig=(j == 1))
                pt2r.rel(pb_, pv)
                if kp == 7:
                    ri, rec, rw = recr.get()
                    trc = kb.op("vector", lambda e: e.reciprocal(out=rec[64:65, :], in_=po[64:65, :]),
                                waits=[pv] + list(rw), sig=True)
                    qi, QT, ki, KT, qk_toks = head_ctx[h]
                    if qt == 3:
                        QTr.rel(qi, pv)
                        KTr.rel(ki, pv)

                    def fin():
                        bc_i, bcb, bcw = PS_P.get()
                        mbc = kb.op("tensor", lambda e: e.matmul(bcb[0:64, :], lhsT=ones_f[64:65, 0:64],
                                                                 rhs=rec[64:65, :], start=True, stop=True),
                                    waits=[trc] + list(bcw) + CW, sig=True)
                        recr.rel(ri, mbc)
                        si, bcs, sw = bcr.get()
                        tbs = kb.op("vector", lambda e: e.tensor_copy(out=bcs[:], in_=bcb[0:64, :]),
                                    waits=[mbc] + list(sw), sig=True)
                        PS_P.rel(bc_i, tbs)
                        oi, on, ow = onr.get()
                        ton = kb.op("vector", lambda e: e.tensor_tensor(out=on[:], in0=po[0:64, :], in1=bcs[:],
                                                                        op=ALU.mult), waits=[tbs] + list(ow), sig=True)
                        PS_O.rel(bo, ton)
                        bcr.rel(si, ton)
                        stk = store(s_attnT[h * 64:(h + 1) * 64, qt * 512:(qt + 1) * 512], on[:], [ton], on_sems[oi])
                        onr.rel(oi, stk)
                    deferred.append(fin)

            items = [(h, qt, kp) for h in range(16) for qt in range(4) for kp in range(NT // 2)]
            prev = None
            for it in items:
                ctx = mla_stage1(it)
                if prev is not None:
                    mla_stage2(*prev)
                prev = (it, ctx)
            mla_stage2(*prev)
            flush_deferred()
            barrier()

        if stage == 5:
            with nc.sbuf_tensor("dbgt5", [128, 8, T], BF16) as dt_:
                sm = kb.new_sem("dbgs5")
                tk = kb.dma("sync", dt_[:], s_attnT.rearrange("(c p) t -> p c t", p=128), sm)
                store(dbg_out.rearrange("(c p) t -> p c t", p=128), dt_[:], [tk])
                finish()
            nc.used_inputs = list(used_inputs)
            return nc

        outproj_ln(I("w_out1"), None, s_attnT, s_x2, 2, s_x3, "d1")

        if stage == 6:
            with nc.sbuf_tensor("dbgt6", [128, NT, D], F32) as dt_:
                sm = kb.new_sem("dbgs6")
                tk = kb.dma("sync", dt_[:], s_x3.rearrange("(t p) n -> p t n", p=128), sm)
                store(dbg_out.rearrange("(t p) n -> p t n", p=128), dt_[:], [tk])
                finish()
            nc.used_inputs = list(used_inputs)
            return nc

        comb = kb.sbuf("comb", [128, NT, NE], F32)
        logits = kb.sbuf("logits", [128, NT, NE], F32)
        mx = kb.sbuf("mx", [128, NT, 8], F32)
        wts = kb.sbuf("wts", [128, 6, NT], F32)
        eq = kb.sbuf("eq", [128, NT, 2, NE], F32)

        def router_compute(scr):
            wrB = scr[:, 0:8, :]
            x3b = [scr[:, 8, :], scr[:, 9, :]]
            x3free = [[], []]
            jr = scr[:, 10:12, :]
            x3s = [kb.new_sem(f"x3s{i}") for i in range(2)]
            jr_tok = [None, None]
            rs = kb.new_sem("rts")
            rtok = kb.dma("sync", wrB.rearrange("p e d -> p (e d)"), I("router_wT")[0, :].partition_broadcast(128), rs)
            lg_tok = None
            n_ = 0
            for t in range(NT):
                xi = t % 2
                x3 = x3b[xi]
                lt = kb.dma("sync", x3, s_x3[t * 128:(t + 1) * 128, :], x3s[xi], waits=x3free[xi])
                x3free[xi] = []
                for e_ in range(NE):
                    lg_tok = kb.op("vector", lambda e, e_=e_: e.scalar_tensor_tensor(
                        out=jr[:, n_ % 2, :], in0=x3, scalar=1.0, in1=wrB[:, e_, :], op0=ALU.mult, op1=ALU.mult,
                        accum_out=logits[:, t, e_:e_ + 1]), waits=[lt, rtok, jr_tok[n_ % 2]], sig=True)
                    jr_tok[n_ % 2] = lg_tok
                    n_ += 1
                x3free[xi].append(lg_tok)
            tm = None
            for t in range(NT):
                tm = kb.op("vector", lambda e: e.max(out=mx[:, t, :], in_=logits[:, t, :]), waits=[lg_tok], sig=True)
            td_ = kb.op("vector", lambda e: e.tensor_tensor(out=wts[:, 0, :], in0=mx[:, :, 1], in1=mx[:, :, 0],
                                                            op=ALU.subtract), waits=[tm], sig=True)
            te2 = kb.op("scalar", lambda e: e.activation(out=wts[:, 1, :], in_=wts[:, 0, :], func=AF.Exp), waits=[td_],
                        sig=True)
            tdn = kb.op("vector", lambda e: e.tensor_scalar(out=wts[:, 2, :], in0=wts[:, 1, :], scalar1=1.0, scalar2=None,
                                                            op0=ALU.add), waits=[te2], sig=True)
            tw1 = kb.op("vector", lambda e: e.reciprocal(out=wts[:, 3, :], in_=wts[:, 2, :]), waits=[tdn], sig=True)
            tw2 = kb.op("vector", lambda e: e.tensor_tensor(out=wts[:, 4, :], in0=wts[:, 1, :], in1=wts[:, 3, :],
                                                            op=ALU.mult), waits=[tw1], sig=True)
            comb_tok = None
            for t in range(NT):
                ea = kb.op("vector", lambda e: e.tensor_scalar(out=eq[:, t, 0, :], in0=logits[:, t, :],
                                                               scalar1=mx[:, t, 0:1], scalar2=wts[:, 3, t:t + 1],
                                                               op0=ALU.is_equal, op1=ALU.mult), waits=[tw2], sig=True)
                eb = kb.op("vector", lambda e: e.tensor_scalar(out=eq[:, t, 1, :], in0=logits[:, t, :],
                                                               scalar1=mx[:, t, 1:2], scalar2=wts[:, 4, t:t + 1],
                                                               op0=ALU.is_equal, op1=ALU.mult), sig=True)
                comb_tok = kb.op("vector", lambda e: e.tensor_tensor(out=comb[:, t, :], in0=eq[:, t, 0, :],
                                                                     in1=eq[:, t, 1, :], op=ALU.add),
                                 waits=[ea, eb], sig=True)
            return comb_tok

        experts = [(I("moe_wg")[e_], I("moe_wu")[e_], I("moe_wd")[e_], e_) for e_ in range(NE)]
        ffn_ln(experts, comb, s_x3, 3, y_out, None, "f1", pre_compute=router_compute)
        if stage == 7:
            with nc.sbuf_tensor("dbgt7", [128, NT, D], F32) as dt_:
                sm = kb.new_sem("dbgs7")
                tk = kb.dma("sync", dt_[:], y_out.rearrange("(t p) n -> p t n", p=128), sm)
                store(dbg_out.rearrange("(t p) n -> p t n", p=128), dt_[:], [tk])
        finish()
    nc.used_inputs = list(used_inputs)
    return nc


def host_inputs(inp):
    f = lambda a: np.ascontiguousarray(np.asarray(a, dtype=np.float32))
    c = host_consts()
    sh = {}
    sh["even_w_in"] = f(inp["even_w_in"][0])
    gu = np.asarray(inp["gla_gate_up"][0], np.float32)
    gbd = np.zeros((32, 512), np.float32)
    gbd[0:16, 0:256] = gu[0]
    gbd[16:32, 256:512] = gu[1]
    sh["gate_bd"] = gbd
    sh["gate_bias"] = f(np.asarray(inp["gla_gate_bias"][0]).reshape(1, 512))
    sh["gla_gain"] = f(np.asarray(inp["gla_norm_gain"][0]).reshape(1, 128))
    sh["diff_lambda"] = f(np.asarray(inp["diff_lambda"][0]).reshape(1, 256))
    sh["diff_gain"] = f(np.asarray(inp["diff_norm_gain"][0]).reshape(1, 128))
    sh["even_w_out"] = f(inp["even_w_out"][0])
    sh["ffn_w_gate"] = f(inp["ffn_w_gate"][0])
    sh["ffn_w_up"] = f(inp["ffn_w_up"][0])
    sh["ffn_w_down"] = f(inp["ffn_w_down"][0])
    w1 = np.asarray(inp["odd_w_in"][0], np.float32)
    sh["odd_w_in"] = f(w1)
    sh["odd_w_in_rot"] = f(np.concatenate([w1[:, 400:416], w1[:, 384:400]], axis=1))
    sh["mla_q_gain"] = f(np.asarray(inp["mla_q_norm_gain"][0]).reshape(1, 256))
    sh["mla_kv_gain"] = f(np.asarray(inp["mla_kv_norm_gain"][0]).reshape(1, 128))
    wq = np.asarray(inp["mla_w_uq"][0], np.float32)
    sh["mla_w_uq"] = f(wq)
    wq3 = wq.reshape(256, 16, 96)
    sh["mla_w_uq_rot"] = f(np.concatenate([wq3[:, :, 80:96], wq3[:, :, 64:80]], axis=2).reshape(256, 512))
    sh["mla_w_ukv"] = f(inp["mla_w_ukv"][0])
    sh["odd_w_out"] = f(inp["odd_w_out"][0])
    sh["router_wT"] = f(np.asarray(inp["router_w"][0]).T.reshape(1, NE * D))
    sh["moe_w_gate"] = f(inp["moe_w_gate"][0])
    sh["moe_w_up"] = f(inp["moe_w_up"][0])
    sh["moe_w_down"] = f(inp["moe_w_down"][0])
    sh["ln_gain"] = f(np.asarray(inp["ln_gain"]).reshape(4, D))
    sh["ln_bias"] = f(np.asarray(inp["ln_bias"]).reshape(4, D))
    table = np.asarray(inp["rel_bias_table"], np.float32)
    kl = np.arange(128)[:, None, None]
    m = np.arange(6)[None, :, None]
    ql = np.arange(512)[None, None, :]
    rel = 128 * (m - 1) + kl - ql
    bidx = _bucket(rel)
    toe = table[bidx]
    sh["toe"] = f(np.transpose(toe, (0, 3, 1, 2)).reshape(128, 4 * 6 * 512))
    sh["far"] = f(np.stack([table[15, :], table[31, :]], axis=1).reshape(1, 8))
    for k in ["RF", "RB", "SU", "SL", "ident", "cosF", "sinF"]:
        sh["c_" + k] = c[k]
    return sh


_NC_CACHE = {}


def kernel(**inputs):
    sh = host_inputs(inputs)
    x = np.asarray(inputs["x"], np.float32)
    B = x.shape[0]
    if "nc" not in _NC_CACHE:
        _NC_CACHE["nc"] = build()
    nc = _NC_CACHE["nc"]
    in_maps = []
    for b in range(B):
        m = dict(sh)
        m["x"] = np.ascontiguousarray(x[b])
        in_maps.append({k: v for k, v in m.items() if k in nc.used_inputs})
    res = run_bass_kernel_spmd(nc, in_maps, core_ids=list(range(B)))
    return np.stack([np.asarray(r["y"], np.float32) for r in res.results], axis=0)
```
